# Optimizing a Trainium2 kernel written in Bass

```python
import jax
import jax.numpy as jnp
from jax import lax
import numpy as np

D_MODEL = 1024
BATCH = 4
SEQ = 8192
DEPTH = 1

GRID_W = 64
NORM_EPS = 1e-6
ATT_HEADS = 8
ATT_KV_HEADS = 2
ATT_HEAD_DIM = 64
ATT_GROUPS = ATT_HEADS // ATT_KV_HEADS
ATT_WIDTH = ATT_HEADS * ATT_HEAD_DIM
ATT_KV_WIDTH = ATT_KV_HEADS * ATT_HEAD_DIM
Q_BLOCK = 128
ROPE_THETA = 10000.0
ROPE_AXIS_DIM = ATT_HEAD_DIM // 2
ML_HEADS = 4
ML_HEAD_DIM = 128
ML_WIDTH = ML_HEADS * ML_HEAD_DIM
ML_CHUNK = 128
ML_CONV = 3
ML_DIRS = 2
MIX_WIDTH = ATT_WIDTH + ML_WIDTH
IN_SPLITS = (ATT_WIDTH, ATT_KV_WIDTH, ATT_KV_WIDTH, ML_WIDTH, ML_WIDTH, ML_WIDTH, ML_WIDTH,
             ML_DIRS * ML_HEADS, ML_DIRS * ML_HEADS)
IN_WIDTH = sum(IN_SPLITS)
PEER_HEADS = 8
PEER_NKEYS = 128
PEER_EXPERTS = PEER_NKEYS * PEER_NKEYS
PEER_KEY_DIM = 128
PEER_TOPK = 16
PEER_TOKEN_BLOCK = 128

kernel_name = "hymba_gqa_mlstm_peer_encoder"


def rms_norm(x, gain):
    xf = x.astype(jnp.float32)
    y = xf * lax.rsqrt(jnp.mean(xf * xf, axis=-1, keepdims=True) + NORM_EPS)
    return (y * gain.astype(jnp.float32)).astype(x.dtype)


def split_columns(proj):
    parts, start = [], 0
    for width in IN_SPLITS:
        parts.append(proj[..., start:start + width])
        start += width
    return parts


def axial_rope_tables(rows):
    row_ids = jnp.repeat(jnp.arange(rows, dtype=jnp.float32), GRID_W)
    col_ids = jnp.tile(jnp.arange(GRID_W, dtype=jnp.float32), rows)
    inv_freq = ROPE_THETA ** (-jnp.arange(0, ROPE_AXIS_DIM, 2, dtype=jnp.float32) / ROPE_AXIS_DIM)
    ang = jnp.concatenate([row_ids[:, None] * inv_freq, col_ids[:, None] * inv_freq], axis=-1)
    return jnp.cos(ang), jnp.sin(ang)


def apply_axial_rope(t, cos, sin):
    tf = t.astype(jnp.float32).reshape(*t.shape[:-1], ATT_HEAD_DIM // 2, 2)
    c = cos[None, :, None, :]
    s = sin[None, :, None, :]
    t0, t1 = tf[..., 0], tf[..., 1]
    out = jnp.stack([t0 * c - t1 * s, t0 * s + t1 * c], axis=-1)
    return out.reshape(t.shape).astype(t.dtype)


def grid_attention(q, k, v, q_gain, k_gain, cos, sin):
    b, s = q.shape[0], q.shape[1]
    q = apply_axial_rope(rms_norm(q, q_gain), cos, sin)
    k = apply_axial_rope(rms_norm(k, k_gain), cos, sin)
    n_blocks = s // Q_BLOCK
    qb = q.reshape(b, n_blocks, Q_BLOCK, ATT_KV_HEADS, ATT_GROUPS, ATT_HEAD_DIM)
    qb = jnp.moveaxis(qb, 1, 0)
    scale = ATT_HEAD_DIM ** -0.5

    def one_block(q_blk):
        scores = jnp.einsum('bqkgd,bskd->bkgqs', q_blk, k).astype(jnp.float32) * scale
        probs = jax.nn.softmax(scores, axis=-1).astype(v.dtype)
        return jnp.einsum('bkgqs,bskd->bqkgd', probs, v)

    out = lax.map(one_block, qb)
    return jnp.moveaxis(out, 0, 1).reshape(b, s, ATT_WIDTH)


def centred_depthwise_conv(x, w, bias):
    pad = ML_CONV // 2
    y = lax.conv_general_dilated(
        x, w[:, None, :].astype(x.dtype), window_strides=(1,), padding=[(pad, pad)],
        dimension_numbers=('NWC', 'WIO', 'NWC'), feature_group_count=x.shape[-1])
    return y + bias


def mlstm_chunkwise(q, k, v, log_i, log_f):
    b, h, s, d = q.shape
    n_chunks = s // ML_CHUNK

    def to_chunks(a):
        return jnp.moveaxis(a.reshape(b, h, n_chunks, ML_CHUNK, *a.shape[3:]), 2, 0)

    xs = (to_chunks(q), to_chunks(k), to_chunks(v), to_chunks(log_i), to_chunks(log_f))
    lower = jnp.tril(jnp.ones((ML_CHUNK, ML_CHUNK), dtype=bool))

    def step(carry, inp):
        c_state, n_state, m_state = carry
        qj, kj, vj, ij, fj = inp
        qf, kf, vf = qj.astype(jnp.float32), kj.astype(jnp.float32), vj.astype(jnp.float32)
        bcum = jnp.cumsum(fj, axis=-1)
        dmat = bcum[..., :, None] - bcum[..., None, :] + ij[..., None, :]
        dmat = jnp.where(lower, dmat, -jnp.inf)
        inter = bcum + m_state[..., None]
        m_row = jnp.maximum(inter, jnp.max(dmat, axis=-1))
        w_intra = jnp.exp(dmat - m_row[..., None])
        w_inter = jnp.exp(inter - m_row)
        qk = jnp.einsum('bhld,bhsd->bhls', qf, kf) * w_intra
        num = (w_inter[..., None] * jnp.einsum('bhld,bhde->bhle', qf, c_state)
               + jnp.einsum('bhls,bhse->bhle', qk, vf))
        den = w_inter * jnp.einsum('bhld,bhd->bhl', qf, n_state) + jnp.sum(qk, axis=-1)
        h_out = num / jnp.maximum(jnp.abs(den), jnp.exp(-m_row))[..., None]
        b_tot = bcum[..., -1]
        w_src = b_tot[..., None] - bcum + ij
        m_new = jnp.maximum(b_tot + m_state, jnp.max(w_src, axis=-1))
        decay = jnp.exp(b_tot + m_state - m_new)
        w_src = jnp.exp(w_src - m_new[..., None])
        c_new = decay[..., None, None] * c_state + jnp.einsum('bhs,bhsd,bhse->bhde', w_src, kf, vf)
        n_new = decay[..., None] * n_state + jnp.einsum('bhs,bhsd->bhd', w_src, kf)
        return (c_new, n_new, m_new), h_out.astype(qj.dtype)

    init = (jnp.zeros((b, h, d, d), jnp.float32), jnp.zeros((b, h, d), jnp.float32),
            jnp.zeros((b, h), jnp.float32))
    _, hs = lax.scan(step, init, xs)
    return jnp.moveaxis(hs, 0, 2).reshape(b, h, s, d)


def mlstm_mixer(q, k, v, o, gi, gf, conv_w, conv_b, i_bias, f_bias, out_gain):
    b, s, _ = q.shape
    qk = jax.nn.silu(centred_depthwise_conv(jnp.concatenate([q, k], axis=-1), conv_w, conv_b))
    q, k = qk[..., :ML_WIDTH], qk[..., ML_WIDTH:]

    def heads(a):
        return a.reshape(b, s, ML_HEADS, ML_HEAD_DIM).transpose(0, 2, 1, 3)

    qh, kh, vh = heads(q), heads(k) * (ML_HEAD_DIM ** -0.5), heads(v)
    log_i = (gi.reshape(b, s, ML_DIRS, ML_HEADS) + i_bias).astype(jnp.float32)
    log_f = jax.nn.log_sigmoid((gf.reshape(b, s, ML_DIRS, ML_HEADS) + f_bias).astype(jnp.float32))
    log_i = jnp.transpose(log_i, (2, 0, 3, 1))
    log_f = jnp.transpose(log_f, (2, 0, 3, 1))
    h_fwd = mlstm_chunkwise(qh, kh, vh, log_i[0], log_f[0])
    rev = lambda a: jnp.flip(a, axis=2)
    h_bwd = rev(mlstm_chunkwise(rev(qh), rev(kh), rev(vh), rev(log_i[1]), rev(log_f[1])))
    hsum = (h_fwd + h_bwd).transpose(0, 2, 1, 3)
    hsum = rms_norm(hsum, out_gain).reshape(b, s, ML_WIDTH)
    return jax.nn.sigmoid(o) * hsum


def peer_ffn(xn, w_query, sub_keys, expert_down, expert_up):
    b, s, dm = xn.shape
    t = b * s
    xt = xn.reshape(t, dm)
    qh = (xt @ w_query).reshape(t, PEER_HEADS, 2, PEER_KEY_DIM)
    half_scores = jnp.einsum('thpd,hpnd->thpn', qh, sub_keys).astype(jnp.float32)
    v_half, i_half = lax.top_k(half_scores, PEER_TOPK)
    cand_score = (v_half[:, :, 0, :, None] + v_half[:, :, 1, None, :]).reshape(t, PEER_HEADS, PEER_TOPK * PEER_TOPK)
    cand_id = (i_half[:, :, 0, :, None] * PEER_NKEYS + i_half[:, :, 1, None, :]).reshape(t, PEER_HEADS, PEER_TOPK * PEER_TOPK)
    top_score, top_pos = lax.top_k(cand_score, PEER_TOPK)
    expert_ids = jnp.take_along_axis(cand_id, top_pos, axis=-1)
    gates = jax.nn.softmax(top_score, axis=-1).astype(xn.dtype)
    n_blocks = t // PEER_TOKEN_BLOCK

    def one_block(args):
        x_blk, id_blk, g_blk = args
        act = jax.nn.gelu(jnp.einsum('td,thkd->thk', x_blk, expert_down[id_blk]), approximate=False)
        return jnp.einsum('thk,thkd->td', g_blk * act, expert_up[id_blk])

    out = lax.map(one_block, (xt.reshape(n_blocks, PEER_TOKEN_BLOCK, dm),
                              expert_ids.reshape(n_blocks, PEER_TOKEN_BLOCK, PEER_HEADS, PEER_TOPK),
                              gates.reshape(n_blocks, PEER_TOKEN_BLOCK, PEER_HEADS, PEER_TOPK)))
    return out.reshape(b, s, dm)


def setup_inputs(seed: int = 0) -> dict:
    key = jax.random.key(seed)
    ks = jax.random.split(key, 16)
    f32 = jnp.float32
    nrm = lambda k, shape, scale: jax.random.normal(k, shape, f32) * scale
    gain = lambda k, shape: 1.0 + 0.02 * jax.random.normal(k, shape, f32)
    f_bias = jnp.linspace(3.0, 6.0, ML_HEADS, dtype=f32)[None, None, :] + nrm(ks[8], (DEPTH, ML_DIRS, ML_HEADS), 0.1)
    return {
        "x": nrm(ks[0], (BATCH, SEQ, D_MODEL), 1.0),
        "norm1_gain": gain(ks[1], (DEPTH, D_MODEL)),
        "w_in": nrm(ks[2], (DEPTH, D_MODEL, IN_WIDTH), D_MODEL ** -0.5),
        "att_q_gain": gain(ks[3], (DEPTH, ATT_HEAD_DIM)),
        "att_k_gain": gain(ks[4], (DEPTH, ATT_HEAD_DIM)),
        "ml_conv_w": nrm(ks[5], (DEPTH, ML_CONV, 2 * ML_WIDTH), ML_CONV ** -0.5),
        "ml_conv_b": nrm(ks[6], (DEPTH, 2 * ML_WIDTH), 0.02),
        "ml_igate_bias": nrm(ks[7], (DEPTH, ML_DIRS, ML_HEADS), 0.1),
        "ml_fgate_bias": f_bias,
        "ml_out_gain": gain(ks[9], (DEPTH, ML_HEADS, ML_HEAD_DIM)),
        "w_out": nrm(ks[10], (DEPTH, MIX_WIDTH, D_MODEL), MIX_WIDTH ** -0.5),
        "norm2_gain": gain(ks[11], (DEPTH, D_MODEL)),
        "peer_w_query": nrm(ks[12], (DEPTH, D_MODEL, PEER_HEADS * 2 * PEER_KEY_DIM), D_MODEL ** -0.5),
        "peer_sub_keys": nrm(ks[13], (DEPTH, PEER_HEADS, 2, PEER_NKEYS, PEER_KEY_DIM), PEER_KEY_DIM ** -0.5),
        "peer_down": nrm(ks[14], (DEPTH, PEER_EXPERTS, D_MODEL), D_MODEL ** -0.5),
        "peer_up": nrm(ks[15], (DEPTH, PEER_EXPERTS, D_MODEL), PEER_HEADS ** -0.5),
    }


def reference(x, norm1_gain, w_in, att_q_gain, att_k_gain, ml_conv_w, ml_conv_b, ml_igate_bias,
              ml_fgate_bias, ml_out_gain, w_out, norm2_gain, peer_w_query, peer_sub_keys, peer_down, peer_up):
    b, s, _ = x.shape
    rows = s // GRID_W
    cos, sin = axial_rope_tables(rows)
    h = x
    for layer in range(DEPTH):
        hn = rms_norm(h, norm1_gain[layer])
        proj = hn @ w_in[layer]
        aq, ak, av, mq, mk, mv, mo, mi, mf = split_columns(proj)
        att = grid_attention(aq.reshape(b, s, ATT_HEADS, ATT_HEAD_DIM),
                             ak.reshape(b, s, ATT_KV_HEADS, ATT_HEAD_DIM),
                             av.reshape(b, s, ATT_KV_HEADS, ATT_HEAD_DIM),
                             att_q_gain[layer], att_k_gain[layer], cos, sin)
        mem = mlstm_mixer(mq, mk, mv, mo, mi, mf, ml_conv_w[layer], ml_conv_b[layer],
                          ml_igate_bias[layer], ml_fgate_bias[layer], ml_out_gain[layer])
        h = h + jnp.concatenate([att, mem], axis=-1) @ w_out[layer]
        hn2 = rms_norm(h, norm2_gain[layer])
        h = h + peer_ffn(hn2, peer_w_query[layer], peer_sub_keys[layer], peer_down[layer], peer_up[layer])
    return h
```

```python
import contextlib
import numpy as np
import ml_dtypes
import concourse.bass as bass
import concourse.mybir as mybir
from concourse.bass_utils import run_bass_kernel_spmd

F32 = mybir.dt.float32
BF16 = mybir.dt.bfloat16
I32 = mybir.dt.int32
U32 = mybir.dt.uint32
ALU = mybir.AluOpType
AF = mybir.ActivationFunctionType
AX = mybir.AxisListType

EPOCH = 12000
N_CH = 24
N_CH_SP = 16
ENGS = ("pe", "act", "dve", "pool", "sp")


class Buf:
    __slots__ = ("w", "r")

    def __init__(self):
        self.w = None
        self.r = []


class PBuf:
    __slots__ = ("last",)

    def __init__(self):
        self.last = {}


class Op:
    __slots__ = ("eng", "fn", "deps", "dma", "idx", "sig", "semi", "semv", "ch")


class Prog:
    def __init__(self, nc):
        self.nc = nc
        self.ops = []
        self.g = contextlib.ExitStack()
        self.p = None
        self.pstart = 0
        self.last_dma = [None] * N_CH
        self.rr = 0
        self.rr_pool = 0
        self.barrier = {}
        self.cnt = {e: 0 for e in ENGS}
        self.esem = {e: [] for e in ENGS}
        self.chsem = [self.g.enter_context(nc.semaphore(f"sd{c}")) for c in range(N_CH)]
        self.chcnt = [0] * N_CH
        self.waited = {e: {} for e in ENGS}
        self.last_eng = {e: None for e in ENGS}
        self.big = self.g.enter_context(nc.psum_tensor("psbig", [128, 4096], F32))
        self.banks = [self.big[:, k * 512:(k + 1) * 512] for k in range(8)]
        self.bk = [PBuf() for _ in range(8)]
        self.nm = 0

    def begin(self):
        self.p = contextlib.ExitStack()

    def sbuf(self, shape, dt, name=None):
        self.nm += 1
        return self.p.enter_context(self.nc.sbuf_tensor(name or f"t{self.nm}", list(shape), dt))

    def gsbuf(self, shape, dt, name=None):
        self.nm += 1
        return self.g.enter_context(self.nc.sbuf_tensor(name or f"g{self.nm}", list(shape), dt))

    def op(self, eng, fn, reads=(), writes=(), psum=(), dma=False):
        o = Op()
        o.eng, o.fn, o.dma = eng, fn, dma
        o.sig, o.semi, o.semv, o.ch = False, None, None, None
        o.idx = len(self.ops)
        deps = set()
        for b in reads:
            if b.w is not None:
                deps.add(b.w)
        for b in writes:
            if b.w is not None:
                deps.add(b.w)
            deps.update(b.r)
        for pb in psum:
            for e2, i2 in pb.last.items():
                if e2 != eng:
                    deps.add(i2)
        if dma:
            if eng == "pool":
                ch = N_CH_SP + self.rr_pool % (N_CH - N_CH_SP)
                self.rr_pool += 1
            else:
                ch = self.rr % N_CH_SP
                self.rr += 1
            o.ch = ch
            if self.last_dma[ch] is not None:
                deps.add(self.last_dma[ch])
            self.last_dma[ch] = o.idx
        if eng in self.barrier:
            deps.update(self.barrier.pop(eng))
        if eng == "pe":
            deps = {d for d in deps if self.ops[d].dma or self.ops[d].eng != "pe"}
        o.deps = deps
        for b in reads:
            b.r.append(o.idx)
        for b in writes:
            b.w = o.idx
            b.r = []
        for pb in psum:
            pb.last[eng] = o.idx
        self.ops.append(o)
        self.last_eng[eng] = o.idx
        return o

    def end(self):
        nc, ops = self.nc, self.ops
        lo = self.pstart
        new = ops[lo:]
        bar = set()
        for e in ENGS:
            if self.last_eng[e] is not None:
                bar.add(self.last_eng[e])
        for c in range(N_CH):
            if self.last_dma[c] is not None:
                bar.add(self.last_dma[c])
        for o in new:
            best, keep = {}, set()
            for d in o.deps:
                od = ops[d]
                if od.dma:
                    keep.add(d)
                elif od.eng not in best or best[od.eng] < d:
                    best[od.eng] = d
            keep.update(best.values())
            o.deps = keep
            for d in keep:
                if d >= lo:
                    ops[d].sig = True
        for d in bar:
            if d >= lo:
                ops[d].sig = True
        for o in new:
            if o.dma:
                self.chcnt[o.ch] += 16
                o.semv = self.chcnt[o.ch]
            elif o.sig:
                self.cnt[o.eng] += 1
                ep = (self.cnt[o.eng] - 1) // EPOCH
                o.semi = ep
                o.semv = self.cnt[o.eng] - ep * EPOCH
                while len(self.esem[o.eng]) <= ep:
                    self.esem[o.eng].append(self.g.enter_context(nc.semaphore(f"s{o.eng}{len(self.esem[o.eng])}")))
        engobj = {"pe": "tensor", "act": "scalar", "dve": "vector", "pool": "gpsimd", "sp": "sync"}
        per = {e: [o for o in new if o.eng == e] for e in ENGS}

        def run(ename, eng):
            waited = self.waited[ename]
            for o in per[ename]:
                for d in sorted(o.deps):
                    od = ops[d]
                    if od.dma:
                        key, sem = ("ch", od.ch), self.chsem[od.ch]
                    else:
                        if od.semv is None:
                            continue
                        key, sem = (od.eng, od.semi), self.esem[od.eng][od.semi]
                    if waited.get(key, 0) >= od.semv:
                        continue
                    waited[key] = od.semv
                    eng.wait_ge(sem, od.semv)
                ins = o.fn(eng)
                if o.dma:
                    ins.then_inc(self.chsem[o.ch], 16)
                elif o.sig:
                    ins.then_inc(self.esem[ename][o.semi], 1)

        with nc.Block() as block:
            for ename in ENGS:
                getattr(block, engobj[ename])(lambda e, n=ename: run(n, e))
        self.p.close()
        self.p = None
        self.pstart = len(ops)
        self.barrier = {e: set(bar) for e in ENGS}

    def finish(self):
        nc = self.nc

        def fin(eng):
            for c in range(N_CH):
                if self.chcnt[c] > 0:
                    eng.wait_ge(self.chsem[c], self.chcnt[c])
        with nc.Block() as block:
            block.sync(fin)
        self.g.close()


def DMA(P, out, in_, reads=(), writes=(), q="sp", slow=False):
    if slow:
        return P.op(q, lambda e, o=out, i=in_: e.dma_start(out=o, in_=i, allow_slow_non_contiguous=True), reads, writes, dma=True)
    return P.op(q, lambda e, o=out, i=in_: e.dma_start(out=o, in_=i), reads, writes, dma=True)


def MM(P, out, lhsT, rhs, start, stop, reads, pb, skip=False):
    if skip:
        return P.op("pe", lambda e: e.matmul(out, lhsT=lhsT, rhs=rhs, start=start, stop=stop, skip_group_check=True), reads, (), [pb])
    return P.op("pe", lambda e: e.matmul(out, lhsT=lhsT, rhs=rhs, start=start, stop=stop), reads, (), [pb])


def TR(P, out, in_, ident, reads, pb):
    return P.op("pe", lambda e: e.transpose(out=out, in_=in_, identity=ident), reads, (), [pb])


def ACT(P, out, in_, func, reads=(), writes=(), psum=(), **kw):
    return P.op("act", lambda e: e.activation(out=out, in_=in_, func=func, **kw), reads, writes, psum)


def TT(P, out, in0, in1, op, reads=(), writes=(), psum=(), eng="dve"):
    return P.op(eng, lambda e: e.tensor_tensor(out=out, in0=in0, in1=in1, op=op), reads, writes, psum)


def TS(P, out, in0, s1, s2, op0, op1=None, reads=(), writes=(), psum=(), eng="dve"):
    if op1 is None:
        return P.op(eng, lambda e: e.tensor_scalar(out=out, in0=in0, scalar1=s1, scalar2=None, op0=op0), reads, writes, psum)
    return P.op(eng, lambda e: e.tensor_scalar(out=out, in0=in0, scalar1=s1, scalar2=s2, op0=op0, op1=op1), reads, writes, psum)


def STT(P, out, in0, scalar, in1, op0, op1, reads=(), writes=(), psum=(), eng="dve"):
    return P.op(eng, lambda e: e.scalar_tensor_tensor(out=out, in0=in0, scalar=scalar, in1=in1, op0=op0, op1=op1),
                reads, writes, psum)


def CP(P, out, in_, reads=(), writes=(), psum=(), eng="dve"):
    if eng == "act":
        return P.op("act", lambda e: e.copy(out=out, in_=in_), reads, writes, psum)
    return P.op(eng, lambda e: e.tensor_copy(out=out, in_=in_), reads, writes, psum)


def MSET(P, ap, val, writes=(), eng="dve"):
    return P.op(eng, lambda e: e.memset(ap, val), (), writes)


def RED(P, out, in_, op, reads=(), writes=(), psum=(), eng="dve"):
    return P.op(eng, lambda e: e.tensor_reduce(out=out, in_=in_, axis=AX.X, op=op), reads, writes, psum)


def RECIP(P, out, in_, reads=(), writes=()):
    return P.op("dve", lambda e: e.reciprocal(out=out, in_=in_), reads, writes)


D = 1024
NCH = 8
EPS = 1e-6
NE = 16384


def build(SH, debug=False):
    T = 2 * SH
    NTT = T // 128
    NOWN = SH // 128
    nc = bass.Bass("TRN2", target_bir_lowering=False)
    kin = "ExternalInput"
    kscr = "ExternalOutput" if debug else "Internal"

    def din(name, shape, dt=F32):
        return nc.dram_tensor(name, list(shape), dt, kind=kin).ap()

    def dscr(name, shape, dt):
        return nc.dram_tensor(name, list(shape), dt, kind=kscr).ap()

    x = din("x", [T, D])
    cosd = din("cos", [T, 32])
    sind = din("sin", [T, 32])
    w_in = din("w_in", [D, 2832])
    n1g = din("n1g", [NCH, 128])
    n2g = din("n2g", [NCH, 128])
    qg = din("qg", [64])
    kg = din("kg", [64])
    convw = din("convw", [3, 1024])
    convb = din("convb", [NCH, 128])
    ibias = din("ibias", [8, 1])
    fbias = din("fbias", [8, 1])
    og = din("og", [512])
    w_out = din("w_out", [D, D])
    wq = din("wq", [D, 2048])
    subk = din("subk", [16, 128, 128])
    down = din("down", [NE, D])
    up = din("up", [NE, D])
    y = nc.dram_tensor("y", [SH, D], F32, kind="ExternalOutput").ap()

    HNT = dscr("HNT", [NCH, 128, T + 2], BF16)
    QR = dscr("QR", [64, 8, SH], BF16)
    KR = dscr("KR", [64, 2, T], BF16)
    V1 = dscr("V1", [128, NTT, 130], BF16)
    MQT = dscr("MQT", [128, 4, SH], BF16)
    MKT = dscr("MKT", [128, 4, T], BF16)
    MK = dscr("MK", [128, NTT, 512], BF16)
    MV = dscr("MV", [128, NTT, 4 * 129], BF16)
    SO = dscr("SO", [128, NOWN, 512], BF16)
    GIA = dscr("GIA", [4, T], F32)
    GFA = dscr("GFA", [4, T], F32)
    GIB = dscr("GIB", [4, T], F32)
    GFB = dscr("GFB", [4, T], F32)
    MIXT = dscr("MIXT", [128, 8, SH], BF16)
    XN2T = dscr("XN2T", [128, 8, SH], BF16)
    DT = dscr("DT", [128, 128, 1024], BF16)
    UB = dscr("UB", [128, 128, 1024], BF16)

    P = Prog(nc)
    BK = P.banks
    bk = P.bk

    identf = P.gsbuf([128, 128], F32, "identf")
    identb = P.gsbuf([128, 128], BF16, "identb")
    iotaf = P.gsbuf([128, 128], F32, "iotaf")
    iotab = P.gsbuf([128, 128], BF16, "iotab")
    epst = P.gsbuf([128, 1], F32, "epst")
    b_const = Buf()

    P.begin()
    iot = P.sbuf([128, 128], F32)
    P.op("pool", lambda e: e.iota(iot[:], pattern=[[1, 128]], base=0, channel_multiplier=-1,
                                   allow_small_or_imprecise_dtypes=True), (), [b_const])
    P.op("dve", lambda e: e.tensor_single_scalar(out=identf[:], in_=iot[:], scalar=0.0, op=ALU.is_equal), [b_const], [b_const])
    CP(P, identb[:], identf[:], [b_const], [b_const])
    P.op("pool", lambda e: e.iota(iotaf[:], pattern=[[1, 128]], base=0, channel_multiplier=0,
                                   allow_small_or_imprecise_dtypes=True), (), [b_const])
    CP(P, iotab[:], iotaf[:], [b_const], [b_const])
    MSET(P, epst[:], EPS, [b_const])
    P.end()

    rot_state = {"i": 0}

    def rot(banks):
        k = banks[rot_state["i"] % len(banks)]
        rot_state["i"] += 1
        return k

    def load_colvec(dram_nx128, n, dst, bdst, bank):
        st = P.sbuf([n, 128], F32)
        bs = Buf()
        DMA(P, st[:], dram_nx128, (), [bs])
        TR(P, BK[bank][:, 0:n], st[:], identf[0:n, 0:n], [bs, b_const], bk[bank])
        CP(P, dst, BK[bank][:, 0:n], (), [bdst], [bk[bank]])

    P.begin()
    g1T = P.sbuf([128, NCH], F32)
    bg1 = Buf()
    load_colvec(n1g, NCH, g1T[:], bg1, 7)
    zt = P.sbuf([128, NCH, 1], BF16)
    bz = Buf()
    MSET(P, zt[:], 0.0, [bz])
    HNTv = HNT.rearrange("c p t -> p c t")
    b_hnt = Buf()
    DMA(P, HNTv[:, :, 0:1], zt[:], [bz], [b_hnt], q="pool", slow=True)
    DMA(P, HNTv[:, :, T + 1:T + 2], zt[:], [bz], [b_hnt], q="pool", slow=True)
    xt = [P.sbuf([128, D], F32) for _ in range(4)]
    bxt = [Buf() for _ in range(4)]
    junk = P.sbuf([128, D], BF16)
    bjunk = Buf()
    xs = [P.sbuf([128, D], BF16) for _ in range(2)]
    bxs = [Buf() for _ in range(2)]
    ss = [P.sbuf([128, 1], F32) for _ in range(2)]
    bss = [Buf() for _ in range(2)]
    HB = 4 if NTT % 4 == 0 else 1
    hnt = [P.sbuf([128, NCH, 128 * HB], BF16) for _ in range(2)]
    bhn = [Buf() for _ in range(2)]

    def norm_transpose(i, src_rows, gT, bgT, dst_dram, banks, store_q):
        s = i % 2
        s4 = i % 4
        DMA(P, xt[s4][:], src_rows, (), [bxt[s4]])
        ACT(P, junk[:], xt[s4][:], AF.Square, [bxt[s4]], [bjunk, bss[s]], accum_out=ss[s][:])
        ACT(P, ss[s][:], ss[s][:], AF.Sqrt, [bss[s], b_const], [bss[s]], bias=epst[:], scale=1.0 / D)
        RECIP(P, ss[s][:], ss[s][:], [bss[s]], [bss[s]])
        ACT(P, xs[s][:], xt[s4][:], AF.Copy, [bxt[s4], bss[s]], [bxs[s]], scale=ss[s][:])
        b = rot(banks)
        pv = BK[b][:].bitcast(BF16).rearrange("p (c t) -> p c t", c=NCH)
        for c in range(NCH):
            TR(P, pv[:, c, :], xs[s][:, c * 128:(c + 1) * 128], identb[:], [bxs[s], b_const], bk[b])
        hg = (i // HB) % 2
        sub = i % HB
        TT(P, hnt[hg][:, :, sub * 128:(sub + 1) * 128], pv, gT[:, :, None].to_broadcast([128, NCH, 128]), ALU.mult, [bgT], [bhn[hg]], [bk[b]])
        if sub == HB - 1:
            i0 = i - (HB - 1)
            DMA(P, HNTv[:, :, 1 + i0 * 128:1 + (i + 1) * 128], hnt[hg][:], [bhn[hg]], [b_hnt], q=store_q)

    C_AQ, C_AK, C_AV = 0, 512, 640
    C_MV, C_MO = 768, 1280
    C_MQ = 1792
    C_MK = 1792 + 1536
    C_GI = C_MK + 1536
    C_GF = C_GI + 8
    NCOL = C_GF + 8
    WBD = dscr("WBD", [128, NCH, NCOL], BF16)
    wbs = P.sbuf([128, NCOL], BF16)
    bwbs = Buf()
    b_wbd = Buf()
    cwb = P.sbuf([128, 3, 1024], F32)
    bcw = Buf()
    DMA(P, cwb[:].rearrange("p a b -> p (a b)"), convw.rearrange("a b -> (a b)").partition_broadcast(128), (), [bcw])
    wst = [P.sbuf([128, 2832], F32) for _ in range(2)]
    bws = [Buf() for _ in range(2)]
    def wprep(c):
        s = c % 2
        DMA(P, wst[s][:], w_in[c * 128:(c + 1) * 128, :], (), [bws[s]])
        CP(P, wbs[:, 0:768], wst[s][:, 0:768], [bws[s]], [bwbs], eng="act")
        CP(P, wbs[:, C_MV:C_MV + 1024], wst[s][:, 1792:2816], [bws[s]], [bwbs], eng="pool")
        CP(P, wbs[:, C_GI:C_GI + 16], wst[s][:, 2816:2832], [bws[s]], [bwbs], eng="pool")
        for tap in range(3):
            TT(P, wbs[:, C_MQ + tap * 512:C_MQ + (tap + 1) * 512], wst[s][:, 768:1280], cwb[:, tap, 0:512], ALU.mult,
               [bws[s], bcw], [bwbs])
            TT(P, wbs[:, C_MK + tap * 512:C_MK + (tap + 1) * 512], wst[s][:, 1280:1792], cwb[:, tap, 512:1024], ALU.mult,
               [bws[s], bcw], [bwbs], eng="pool")
        DMA(P, WBD[:, c, :], wbs[:], [bwbs], [b_wbd], q="pool")

    wdone = 0
    for i in range(NTT):
        while wdone < NCH and wdone * NTT <= i * NCH:
            wprep(wdone)
            wdone += 1
        norm_transpose(i, x[i * 128:(i + 1) * 128, :], g1T, bg1, HNTv[:, :, 1 + i * 128:1 + (i + 1) * 128], [0, 1, 2], "pool")
    while wdone < NCH:
        wprep(wdone)
        wdone += 1
    P.end()

    P.begin()
    wb = P.sbuf([128, NCH, NCOL], BF16)
    bwb = Buf()
    DMA(P, wb[:], WBD, [b_wbd], [bwb])
    cbT = P.sbuf([128, NCH], F32)
    bcb = Buf()
    load_colvec(convb, NCH, cbT[:], bcb, 7)
    ib = P.sbuf([8, 1], F32)
    fbn = P.sbuf([8, 1], F32)
    bgb = Buf()
    DMA(P, ib[:], ibias, (), [bgb])
    DMA(P, fbn[:], fbias, (), [bgb])
    TS(P, fbn[:], fbn[:], -1.0, None, ALU.mult, reads=[bgb], writes=[bgb])
    gq = P.sbuf([128, 64], F32)
    gk = P.sbuf([128, 64], F32)
    bgq = Buf()
    DMA(P, gq[:], qg.partition_broadcast(128), (), [bgq])
    DMA(P, gk[:], kg.partition_broadcast(128), (), [bgq])
    TS(P, gq[:], gq[:], 0.125, None, ALU.mult, reads=[bgq], writes=[bgq])

    TW = 512 if SH >= 512 else SH
    NSUB = TW // 128
    hn = [P.sbuf([128, NCH, TW + 2], BF16) for _ in range(2)]
    bhn1 = [Buf() for _ in range(2)]
    cs = [P.sbuf([128, 32], F32) for _ in range(2)]
    sn = [P.sbuf([128, 32], F32) for _ in range(2)]
    bcs = [Buf() for _ in range(2)]
    sq = [P.sbuf([128, 640], F32) for _ in range(2)]
    bsq = [Buf() for _ in range(2)]
    ssq = [P.sbuf([128, 10], F32) for _ in range(2)]
    bssq = [Buf() for _ in range(2)]
    qn = [P.sbuf([128, 640], F32) for _ in range(2)]
    bqn = [Buf() for _ in range(2)]
    ta = [P.sbuf([128, 10, 32], F32) for _ in range(2)]
    tb_ = [P.sbuf([128, 10, 32], F32) for _ in range(2)]
    bta = [Buf() for _ in range(2)]
    btb = [Buf() for _ in range(2)]
    qr = [P.sbuf([128, 640], BF16) for _ in range(2)]
    bqr = [Buf() for _ in range(2)]
    mhalf = P.sbuf([128, 10], F32)
    bmh = Buf()
    MSET(P, mhalf[:], -0.5, [bmh])
    qT = [P.sbuf([64, 8, TW], BF16) for _ in range(2)]
    bqT = [Buf() for _ in range(2)]
    kTt = [P.sbuf([64, 2, TW], BF16) for _ in range(2)]
    bkT = [Buf() for _ in range(2)]
    v1 = [P.sbuf([128, NSUB, 130], BF16) for _ in range(2)]
    bv1 = [Buf() for _ in range(2)]
    for s in range(2):
        MSET(P, v1[s][:], 1.0, [bv1[s]])
    mv = [P.sbuf([128, NSUB, 4 * 129], BF16) for _ in range(2)]
    bmv = [Buf() for _ in range(2)]
    for s in range(2):
        MSET(P, mv[s][:], 1.0, [bmv[s]])
    so = [P.sbuf([128, NSUB, 512], BF16) for _ in range(2)]
    bso = [Buf() for _ in range(2)]
    mqT = [P.sbuf([128, 4, TW], BF16) for _ in range(2)]
    bmq = [Buf() for _ in range(2)]
    mkf = P.sbuf([128, TW], F32)
    bmkf = Buf()
    mkT = [P.sbuf([128, 4, TW], BF16) for _ in range(2)]
    bmk = [Buf() for _ in range(2)]
    mkt = [P.sbuf([128, NSUB, 512], BF16) for _ in range(2)]
    bmkt = [Buf() for _ in range(2)]
    gi = [P.sbuf([8, TW], F32) for _ in range(2)]
    gf = [P.sbuf([8, TW], F32) for _ in range(2)]
    bgi = [Buf() for _ in range(2)]
    bgf = [Buf() for _ in range(2)]
    b_p1out = Buf()
    ROT1 = [0, 1, 2, 3, 4, 5, 6, 7]

    def qk_norm_rope(pbank, col0, nh, gain, out_cols, own_s, pr, o0):
        n = nh * 64
        src = BK[pbank][:, col0:col0 + n]
        sqv = sq[pr][:, o0:o0 + n]
        ssv = ssq[pr][:, o0 // 64:o0 // 64 + nh]
        ACT(P, sqv, src, AF.Square, (), [bsq[pr]], [bk[pbank]])
        RED(P, ssv, sqv.rearrange("p (h d) -> p h d", d=64), ALU.add, [bsq[pr]], [bssq[pr]])
        TS(P, ssv, ssv, 1.0 / 64, EPS, ALU.mult, ALU.add, reads=[bssq[pr]], writes=[bssq[pr]], eng="pool")
        TT(P, ssv, ssv, mhalf[:, 0:nh], ALU.pow, [bssq[pr], bmh], [bssq[pr]], eng="pool")
        qv = qn[pr][:, o0:o0 + n].rearrange("p (h d) -> p h d", d=64)
        TT(P, qv, src.rearrange("p (h d) -> p h d", d=64), ssv[:, :, None].to_broadcast([128, nh, 64]), ALU.mult,
           [bssq[pr]], [bqn[pr]], [bk[pbank]])
        TT(P, qv, qv, gain[:, None, :].to_broadcast([128, nh, 64]), ALU.mult, [bqn[pr], bgq], [bqn[pr]])
        q4 = qn[pr][:, o0:o0 + n].rearrange("p (h i two) -> p h i two", two=2, i=32)
        t0, t1 = q4[:, :, :, 0], q4[:, :, :, 1]
        cb = cs[own_s][:, 0:32][:, None, :].to_broadcast([128, nh, 32])
        sb = sn[own_s][:, 0:32][:, None, :].to_broadcast([128, nh, 32])
        o4 = qr[pr][:, out_cols:out_cols + n].rearrange("p (h i two) -> p h i two", two=2, i=32)
        h0 = o0 // 64
        A, B = ta[pr][:, h0:h0 + nh, :], tb_[pr][:, h0:h0 + nh, :]
        TT(P, A, t0, cb, ALU.mult, [bqn[pr], bcs[own_s]], [bta[pr]])
        TT(P, B, t1, sb, ALU.mult, [bqn[pr], bcs[own_s]], [btb[pr]], eng="pool")
        TT(P, o4[:, :, :, 0], A, B, ALU.subtract, [bta[pr], btb[pr]], [bqr[pr]])
        TT(P, A, t0, sb, ALU.mult, [bqn[pr], bcs[own_s]], [bta[pr]])
        TT(P, B, t1, cb, ALU.mult, [bqn[pr], bcs[own_s]], [btb[pr]], eng="pool")
        TT(P, o4[:, :, :, 1], A, B, ALU.add, [bta[pr], btb[pr]], [bqr[pr]])

    NTW = T // TW
    NTW_OWN = SH // TW
    csi = 0
    pending_tr = []
    for tt in range(NTW):
        s = tt % 2
        own = tt < NTW_OWN
        DMA(P, hn[s][:], HNTv[:, :, tt * TW:tt * TW + TW + 2], [b_hnt], [bhn1[s]])
        for su in range(NSUB):
            tok0 = tt * TW + su * 128
            c_s = csi % 2
            csi += 1
            DMA(P, cs[c_s][:], cosd[tok0:tok0 + 128, :], (), [bcs[c_s]])
            DMA(P, sn[c_s][:], sind[tok0:tok0 + 128, :], (), [bcs[c_s]])
            lcols = slice(1 + su * 128, 1 + (su + 1) * 128)
            if own:
                bq = rot(ROT1)
                for c in range(NCH):
                    MM(P, BK[bq][:, 0:512], hn[s][:, c, lcols], wb[:, c, C_AQ:C_AQ + 512], c == 0, c == NCH - 1, [bhn1[s], bwb], bk[bq])
            bkv = rot(ROT1)
            for c in range(NCH):
                MM(P, BK[bkv][:, 0:256], hn[s][:, c, lcols], wb[:, c, C_AK:C_AK + 256], c == 0, c == NCH - 1, [bhn1[s], bwb], bk[bkv])
            CP(P, v1[s][:, su, :].rearrange("p (g e) -> p g e", e=65)[:, :, 0:64],
               BK[bkv][:, 128:256].rearrange("p (g e) -> p g e", e=64), (), [bv1[s]], [bk[bkv]], eng="act")
            pr = c_s
            if own:
                qk_norm_rope(bq, 0, 8, gq, 0, c_s, pr, 0)
            qk_norm_rope(bkv, 0, 2, gk, 512, c_s, pr, 512)

            def do_tr(s=s, su=su, pr=pr, own=own):
                bt = rot(ROT1)
                ptv = BK[bt][:].bitcast(BF16)
                if own:
                    for h in range(8):
                        TR(P, ptv[0:64, h * 128:(h + 1) * 128], qr[pr][:, h * 64:(h + 1) * 64], identb[:], [bqr[pr], b_const], bk[bt])
                    CP(P, qT[s][:, :, su * 128:(su + 1) * 128], ptv[0:64, :].rearrange("p (h t) -> p h t", t=128), (), [bqT[s]], [bk[bt]], eng="act")
                    bt = rot(ROT1)
                    ptv = BK[bt][:].bitcast(BF16)
                for g in range(2):
                    TR(P, ptv[0:64, g * 128:(g + 1) * 128], qr[pr][:, 512 + g * 64:512 + (g + 1) * 64], identb[:], [bqr[pr], b_const], bk[bt])
                CP(P, kTt[s][:, :, su * 128:(su + 1) * 128], ptv[0:64, 0:256].rearrange("p (h t) -> p h t", t=128), (), [bkT[s]], [bk[bt]], eng="act")
            if pending_tr:
                pending_tr.pop()()
            pending_tr.append(do_tr)
            bm = rot(ROT1)
            for c in range(NCH):
                MM(P, BK[bm][:, 0:512], hn[s][:, c, lcols], wb[:, c, C_MV:C_MV + 512], c == 0, c == NCH - 1, [bhn1[s], bwb], bk[bm])
            CP(P, mv[s][:, su, :].rearrange("p (h e) -> p h e", e=129)[:, :, 0:128],
               BK[bm][:, 0:512].rearrange("p (h e) -> p h e", e=128), (), [bmv[s]], [bk[bm]], eng="act")
            if own:
                bo = rot(ROT1)
                for c in range(NCH):
                    MM(P, BK[bo][:, 0:512], hn[s][:, c, lcols], wb[:, c, C_MO:C_MO + 512], c == 0, c == NCH - 1, [bhn1[s], bwb], bk[bo])
                ACT(P, so[s][:, su, :], BK[bo][:, 0:512], AF.Sigmoid, (), [bso[s]], [bk[bo]])
        if pending_tr:
            pending_tr.pop()()
        if own:
            DMA(P, QR[:, :, tt * TW:(tt + 1) * TW], qT[s][:], [bqT[s]], [b_p1out], q="pool")
            DMA(P, SO[:, tt * NSUB:(tt + 1) * NSUB, :], so[s][:], [bso[s]], [b_p1out], q="pool")
        DMA(P, KR[:, :, tt * TW:(tt + 1) * TW], kTt[s][:], [bkT[s]], [b_p1out], q="pool")
        DMA(P, V1[:, tt * NSUB:(tt + 1) * NSUB, :], v1[s][:], [bv1[s]], [b_p1out], q="pool")
        DMA(P, MV[:, tt * NSUB:(tt + 1) * NSUB, :], mv[s][:], [bmv[s]], [b_p1out], q="pool")
        for j in range(8):
            isq = j < 4
            if isq and not own:
                continue
            base = C_MQ if isq else C_MK
            jj = j % 4
            bc = rot(ROT1)
            n = 0
            for tap in range(3):
                for c in range(NCH):
                    MM(P, BK[bc][:, 0:TW], wb[:, c, base + tap * 512 + jj * 128:base + tap * 512 + (jj + 1) * 128],
                       hn[s][:, c, tap:tap + TW], n == 0, n == 23, [bhn1[s], bwb], bk[bc])
                    n += 1
            if isq:
                ACT(P, mqT[s][:, jj, :], BK[bc][:, 0:TW], AF.Silu, [bcb], [bmq[s]], [bk[bc]], bias=cbT[:, j:j + 1])
            else:
                ACT(P, mkf[:], BK[bc][:, 0:TW], AF.Silu, [bcb], [bmkf], [bk[bc]], bias=cbT[:, j:j + 1])
                TS(P, mkT[s][:, jj, :], mkf[:], 128.0 ** -0.5, None, ALU.mult, reads=[bmkf], writes=[bmk[s]])
        for su in range(NSUB):
            bt = rot(ROT1)
            ptv = BK[bt][:].bitcast(BF16)
            for h in range(4):
                TR(P, ptv[:, h * 128:(h + 1) * 128], mkT[s][:, h, su * 128:(su + 1) * 128], identb[:], [bmk[s], b_const], bk[bt])
            CP(P, mkt[s][:, su, :], ptv[:, 0:512], (), [bmkt[s]], [bk[bt]], eng="act")
        if own:
            DMA(P, MQT[:, :, tt * TW:(tt + 1) * TW], mqT[s][:], [bmq[s]], [b_p1out], q="pool")
        DMA(P, MKT[:, :, tt * TW:(tt + 1) * TW], mkT[s][:], [bmk[s]], [b_p1out], q="pool")
        DMA(P, MK[:, tt * NSUB:(tt + 1) * NSUB, :], mkt[s][:], [bmkt[s]], [b_p1out], q="pool")
        bgp = rot(ROT1)
        for c in range(NCH):
            MM(P, BK[bgp][0:8, 0:TW], wb[:, c, C_GI:C_GI + 8], hn[s][:, c, 1:1 + TW], c == 0, c == NCH - 1, [bhn1[s], bwb], bk[bgp])
        ACT(P, gi[s][:], BK[bgp][0:8, 0:TW], AF.Identity, [bgb], [bgi[s]], [bk[bgp]], bias=ib[:])
        bgp = rot(ROT1)
        for c in range(NCH):
            MM(P, BK[bgp][0:8, 0:TW], wb[:, c, C_GF:C_GF + 8], hn[s][:, c, 1:1 + TW], c == 0, c == NCH - 1, [bhn1[s], bwb], bk[bgp])
        ACT(P, gf[s][:], BK[bgp][0:8, 0:TW], AF.Exp, [bgb], [bgf[s]], [bk[bgp]], bias=fbn[:], scale=-1.0)
        ACT(P, gf[s][:], gf[s][:], AF.Ln, [bgf[s]], [bgf[s]], bias=1.0)
        TS(P, gf[s][:], gf[s][:], -1.0, None, ALU.mult, reads=[bgf[s]], writes=[bgf[s]])
        DMA(P, GIA[:, tt * TW:(tt + 1) * TW], gi[s][0:4, :], [bgi[s]], [b_p1out], q="pool")
        DMA(P, GFA[:, tt * TW:(tt + 1) * TW], gf[s][0:4, :], [bgf[s]], [b_p1out], q="pool")
        DMA(P, GIB[:, tt * TW:(tt + 1) * TW], gi[s][4:8, :], [bgi[s]], [b_p1out], q="pool")
        DMA(P, GFB[:, tt * TW:(tt + 1) * TW], gf[s][4:8, :], [bgf[s]], [b_p1out], q="pool")
    P.end()

    P.begin()
    RT2 = (NTT % 2 == 0)
    KRs = P.sbuf([128, 2, NTT // 2, 128], BF16) if RT2 else P.sbuf([64, 2, T], BF16)
    V1s = P.sbuf([128, NTT, 130], BF16)
    VpA = P.sbuf([128, NTT, 2, 128], BF16)
    VpB = P.sbuf([128, NTT, 2, 128], BF16)
    bld = Buf()
    bvp = Buf()
    for g in range(2):
        if RT2:
            kv = KR[:, g, :].rearrange("d (m two c) -> d m two c", two=2, c=128)
            DMA(P, KRs[0:64, g, :, :], kv[:, :, 0, :], (), [bld])
            DMA(P, KRs[64:128, g, :, :], kv[:, :, 1, :], (), [bld])
        else:
            DMA(P, KRs[:, g, :], KR[:, g, :], (), [bld])
    DMA(P, V1s[:], V1, (), [bld])
    MSET(P, VpA[:], 0.0, [bvp])
    MSET(P, VpB[:], 0.0, [bvp], eng="pool")
    for g in range(2):
        CP(P, VpA[:, :, g, 0:65], V1s[:, :, g * 65:(g + 1) * 65], [bld, bvp], [bvp])
        CP(P, VpB[:, :, g, 64:128], V1s[:, :, g * 65:g * 65 + 64], [bld, bvp], [bvp], eng="pool")
        CP(P, VpB[:, :, g, 0:1], V1s[:, :, g * 65 + 64:g * 65 + 65], [bld, bvp], [bvp], eng="pool")
    onesA = P.sbuf([128, 64], F32)
    onesB = P.sbuf([1, 128], F32)
    bones = Buf()
    MSET(P, onesA[:], 1.0, [bones])
    MSET(P, onesB[:], 0.0, [bones])
    MSET(P, onesB[:, 64:128], 1.0, [bones])
    QB = 512 if SH >= 512 else SH
    NQB = SH // QB
    QRs = [P.sbuf([128, 8, QB], BF16) for _ in range(2)]
    bqr = [Buf() for _ in range(2)]
    KB = 2 if NTT % 2 == 0 else 1
    pt = [P.sbuf([128, KB, QB], BF16) for _ in range(3)]
    bpt = [Buf() for _ in range(3)]
    rcp = [P.sbuf([128, QB], F32) for _ in range(2)]
    brcp = [Buf() for _ in range(2)]
    rbs = [P.sbuf([128, QB], F32) for _ in range(2)]
    brbs = [Buf() for _ in range(2)]
    mixs = [P.sbuf([128, 4, QB], BF16) for _ in range(2)]
    bmix = [Buf() for _ in range(2)]
    b_mixt = Buf()
    OBK = 6
    RBK = 7
    dst = [P.sbuf([128, 1024], F32) for _ in range(2)]
    bdst = [Buf() for _ in range(2)]
    dbf = [P.sbuf([128, 1024], BF16) for _ in range(2)]
    bdbf = [Buf() for _ in range(2)]
    dT = [P.sbuf([128, 1024], BF16) for _ in range(2)]
    bdT = [Buf() for _ in range(2)]
    ust = [P.sbuf([128, 1024], F32) for _ in range(2)]
    bust = [Buf() for _ in range(2)]
    ubf = [P.sbuf([128, 1024], BF16) for _ in range(2)]
    bubf = [Buf() for _ in range(2)]
    b_tab = Buf()

    def prep_block(i):
        s = i % 2
        DMA(P, dst[s][:], down[i * 128:(i + 1) * 128, :], (), [bdst[s]])
        DMA(P, ust[s][:], up[i * 128:(i + 1) * 128, :], (), [bust[s]])
        CP(P, dbf[s][:], dst[s][:], [bdst[s]], [bdbf[s]], eng="dve")
        pv = BK[RBK][:].bitcast(BF16)
        for c in range(8):
            TR(P, pv[:, c * 128:(c + 1) * 128], dbf[s][:, c * 128:(c + 1) * 128], identb[:], [bdbf[s], b_const], bk[RBK])
        CP(P, dT[s][:], pv, (), [bdT[s]], [bk[RBK]])
        DMA(P, DT[:, i, :], dT[s][:], [bdT[s]], [b_tab], q="pool")
        CP(P, ubf[s][:], ust[s][:], [bust[s]], [bubf[s]], eng="dve")
        DMA(P, UB[:, i, :], ubf[s][:], [bubf[s]], [b_tab], q="pool")

    NKB = NTT // KB
    iters = [(qb, h, kb) for qb in range(NQB) for h in range(8) for kb in range(NKB)]
    NIT = len(iters)

    def load_q(qb):
        for h in range(8):
            DMA(P, QRs[qb % 2][0:64, h, :], QR[:, h, qb * QB:(qb + 1) * QB], (), [bqr[qb % 2]])
            if RT2:
                DMA(P, QRs[qb % 2][64:128, h, :], QR[:, h, qb * QB:(qb + 1) * QB], (), [bqr[qb % 2]])

    def emit_S(n):
        qb, h, kb = iters[n]
        g = h // 4
        p_ = n % 3
        for j in range(KB):
            kc = kb * KB + j
            sb_ = 2 * p_ + j
            if RT2:
                rows = slice(64 * (kc % 2), 64 * (kc % 2) + 64)
                MM(P, BK[sb_][:, 0:QB], KRs[rows, g, kc // 2, :], QRs[qb % 2][rows, h, :], True, True, [bld, bqr[qb % 2]], bk[sb_])
            else:
                MM(P, BK[sb_][:, 0:QB], KRs[:, g, kc * 128:(kc + 1) * 128], QRs[qb % 2][0:64, h, :], True, True, [bld, bqr[qb % 2]], bk[sb_])
        if KB == 2 and QB == 512:
            src = P.big[:, 2 * p_ * 512:(2 * p_ + 2) * 512]
            ACT(P, pt[p_][:].rearrange("p k q -> p (k q)"), src, AF.Exp, (), [bpt[p_]], [bk[2 * p_], bk[2 * p_ + 1]])
        else:
            for j in range(KB):
                ACT(P, pt[p_][:, j, :], BK[2 * p_ + j][:, 0:QB], AF.Exp, (), [bpt[p_]], [bk[2 * p_ + j]])

    def emit_PV(n):
        qb, h, kb = iters[n]
        g = h // 4
        hi = qb * 8 + h
        ob = OBK
        Vp = VpA if h % 2 == 0 else VpB
        p_ = n % 3
        for j in range(KB):
            kc = kb * KB + j
            MM(P, BK[ob][:, 0:QB], Vp[:, kc, g, :], pt[p_][:, j, :], kc == 0, kc == NTT - 1, [bpt[p_], bvp], bk[ob])
        if kb == NKB - 1:
            r_ = hi % 2
            a_s = qb % 2
            rb = RBK
            if h % 2 == 0:
                P.op("dve", lambda e, o=rcp[r_][64:65, :], i=BK[ob][64:65, 0:QB]: e.reciprocal(out=o, in_=i), (), [brcp[r_]], [bk[ob]])
                MM(P, BK[rb][0:64, 0:QB], onesA[64:65, 0:64], rcp[r_][64:65, :], True, True, [brcp[r_], bones], bk[rb])
                rows = slice(0, 64)
            else:
                P.op("dve", lambda e, o=rcp[r_][0:1, :], i=BK[ob][0:1, 0:QB]: e.reciprocal(out=o, in_=i), (), [brcp[r_]], [bk[ob]])
                MM(P, BK[rb][:, 0:QB], onesB[0:1, :], rcp[r_][0:1, :], True, True, [brcp[r_], bones], bk[rb])
                rows = slice(64, 128)
            CP(P, rbs[r_][rows, :], BK[rb][rows, 0:QB], (), [brbs[r_]], [bk[rb]])
            TT(P, mixs[a_s][rows, h // 2, :], BK[ob][rows, 0:QB], rbs[r_][rows, :], ALU.mult, [brbs[r_]], [bmix[a_s]], [bk[ob]])
            if h == 7:
                DMA(P, MIXT[:, 0:4, qb * QB:(qb + 1) * QB], mixs[a_s][:], [bmix[a_s]], [b_mixt], q="pool")

    load_q(0)
    if NQB > 1:
        load_q(1)
    for n in range(min(2, NIT)):
        emit_S(n)
    nprep = 0
    for n in range(NIT):
        qb, h, kb = iters[n]
        if n + 2 < NIT:
            emit_S(n + 2)
        emit_PV(n)
        if h == 7 and kb == NKB - 1 and qb + 2 < NQB:
            load_q(qb + 2)
        while nprep < 128 and nprep * NIT <= n * 128:
            prep_block(nprep)
            nprep += 1
    while nprep < 128:
        prep_block(nprep)
        nprep += 1
    P.end()

    P.begin()
    maskA = P.sbuf([128, 128], F32)
    maskB = P.sbuf([128, 128], F32)
    bmask = Buf()
    dif = P.sbuf([128, 128], F32)
    P.op("pool", lambda e: e.iota(dif[:], pattern=[[1, 128]], base=0, channel_multiplier=-1,
                                   allow_small_or_imprecise_dtypes=True), (), [bmask])
    P.op("dve", lambda e: e.tensor_single_scalar(out=maskA[:], in_=dif[:], scalar=0.0, op=ALU.is_ge), [bmask], [bmask])
    P.op("dve", lambda e: e.tensor_single_scalar(out=maskB[:], in_=dif[:], scalar=0.0, op=ALU.is_le), [bmask], [bmask])
    ogb = P.sbuf([128, 512], F32)
    bog = Buf()
    DMA(P, ogb[:], og.partition_broadcast(128), (), [bog])
    ones4 = P.sbuf([4, 128], F32)
    MSET(P, ones4[:], 1.0, [bog])

    ones1 = P.sbuf([1, 128], F32)
    MSET(P, ones1[:], 1.0, [bog])

    def gate_prep(GI_d, GF_d, NC, reverse):
        rows = 4 * NC
        RT = min(128, rows)
        ntile = rows // RT
        hpt = RT // NC
        betaT = P.sbuf([128, rows], F32)
        clmT = P.sbuf([128, rows], F32)
        decB = P.sbuf([128, rows], F32)
        bT = Buf()
        for k in range(ntile):
            h0 = k * hpt
            gi_ = P.sbuf([RT, 128], F32)
            gf_ = P.sbuf([RT, 128], F32)
            bb = Buf()
            for hh in range(hpt):
                DMA(P, gi_[hh * NC:(hh + 1) * NC, :], GI_d[h0 + hh, 0:NC * 128].rearrange("(c t) -> c t", t=128), (), [bb])
                DMA(P, gf_[hh * NC:(hh + 1) * NC, :], GF_d[h0 + hh, 0:NC * 128].rearrange("(c t) -> c t", t=128), (), [bb])
            one_ = P.sbuf([RT, 128], F32)
            MSET(P, one_[:], 1.0, [bb])
            bc_ = P.sbuf([RT, 128], F32)
            if reverse:
                P.op("dve", lambda e, o=bc_[:, ::-1], a=one_[:], b=gf_[:, ::-1]: e.tensor_tensor_scan(out=o, data0=a, data1=b, initial=0.0,
                                                                                                  op0=ALU.mult, op1=ALU.add), [bb], [bb])
            else:
                P.op("dve", lambda e, o=bc_[:], a=one_[:], b=gf_[:]: e.tensor_tensor_scan(out=o, data0=a, data1=b, initial=0.0,
                                                                                          op0=ALU.mult, op1=ALU.add), [bb], [bb])
            a_ = P.sbuf([RT, 128], F32)
            TT(P, a_[:], gi_[:], bc_[:], ALU.subtract, [bb], [bb])
            amax = P.sbuf([RT, 1], F32)
            RED(P, amax[:], a_[:], ALU.max, [bb], [bb])
            btc = bc_[:, 0:1] if reverse else bc_[:, 127:128]
            pb_ = rot([5, 6])
            TR(P, BK[pb_][0:1, 0:RT], amax[:, 0:1], identf[0:RT, 0:RT], [bb, b_const], bk[pb_])
            TR(P, BK[pb_][0:1, 128:128 + RT], btc, identf[0:RT, 0:RT], [bb, b_const], bk[pb_])
            row = P.sbuf([1, 2, 128], F32)
            CP(P, row[:, :, 0:RT], BK[pb_][0:1, 0:256].rearrange("p (a r) -> p a r", a=2)[:, :, 0:RT], (), [bb], [bk[pb_]])
            mnext = P.sbuf([1, RT], F32)
            for hh in range(hpt):
                sl = slice(hh * NC, (hh + 1) * NC)
                if reverse:
                    P.op("dve", lambda e, o=mnext[:, sl][:, ::-1], a=row[:, 0, sl][:, ::-1], b=row[:, 1, sl][:, ::-1]:
                         e.tensor_tensor_scan(out=o, data0=a, data1=b, initial=0.0, op0=ALU.max, op1=ALU.add), [bb], [bb])
                else:
                    P.op("dve", lambda e, o=mnext[:, sl], a=row[:, 0, sl], b=row[:, 1, sl]:
                         e.tensor_tensor_scan(out=o, data0=a, data1=b, initial=0.0, op0=ALU.max, op1=ALU.add), [bb], [bb])
            Mc = P.sbuf([1, RT], F32)
            TT(P, Mc[:], mnext[:], row[:, 1, 0:RT], ALU.subtract, [bb], [bb])
            mst = P.sbuf([1, RT], F32)
            MSET(P, mst[:], 0.0, [bb])
            if NC > 1:
                for hh in range(hpt):
                    if reverse:
                        CP(P, mst[:, hh * NC:(hh + 1) * NC - 1], mnext[:, hh * NC + 1:(hh + 1) * NC], [bb], [bb])
                    else:
                        CP(P, mst[:, hh * NC + 1:(hh + 1) * NC], mnext[:, hh * NC:(hh + 1) * NC - 1], [bb], [bb])
            dec = P.sbuf([1, RT], F32)
            TT(P, dec[:], mst[:], Mc[:], ALU.subtract, [bb], [bb])
            ACT(P, dec[:], dec[:], AF.Exp, [bb], [bb])
            TS(P, Mc[:], Mc[:], -1.0, None, ALU.mult, reads=[bb], writes=[bb])
            pb_ = rot([5, 6])
            TR(P, BK[pb_][0:RT, 0:1], Mc[0:1, 0:RT], identf[0:1, 0:1], [bb, b_const], bk[pb_])
            nMc = P.sbuf([RT, 1], F32)
            CP(P, nMc[:], BK[pb_][0:RT, 0:1], (), [bb], [bk[pb_]])
            beta = P.sbuf([RT, 128], F32)
            ACT(P, beta[:], a_[:], AF.Exp, [bb], [bb], bias=nMc[:])
            clm = P.sbuf([RT, 128], F32)
            ACT(P, clm[:], bc_[:], AF.Exp, [bb], [bb], bias=nMc[:], scale=-1.0)
            for src, dstT in ((beta, betaT), (clm, clmT)):
                pb_ = rot([5, 6])
                TR(P, BK[pb_][:, 0:RT], src[:], identf[0:RT, 0:RT], [bb, b_const], bk[pb_])
                CP(P, dstT[:, k * RT:(k + 1) * RT], BK[pb_][:, 0:RT], (), [bT], [bk[pb_]])
            pb_ = rot([5, 6])
            MM(P, BK[pb_][:, 0:RT], ones1[:], dec[:], True, True, [bb, bog], bk[pb_])
            CP(P, decB[:, k * RT:(k + 1) * RT], BK[pb_][:, 0:RT], (), [bT], [bk[pb_]])
        return betaT, clmT, decB, bT, NC

    betaA, clmA, decA, bTA, NCA = gate_prep(GIA, GFA, NOWN, False)
    betaB, clmB, decB_, bTB, NCB = gate_prep(GIB, GFB, NTT, True)

    U = {}
    for d_ in "AB":
        for h in range(4):
            u = P.sbuf([128, 129], F32)
            bu = Buf()
            MSET(P, u[:], 0.0, [bu])
            U[(d_, h)] = (u, bu)
    hA = P.sbuf([128, NOWN, 512], F32)
    bhA = [Buf() for _ in range(NOWN)]
    NB = 3
    ld = {}
    for d_ in "AB":
        ld[d_] = dict(
            q=[P.sbuf([128, 4, 128], BF16) for _ in range(NB)], k=[P.sbuf([128, 4, 128], BF16) for _ in range(NB)],
            kt=[P.sbuf([128, 512], BF16) for _ in range(NB)], v=[P.sbuf([128, 4 * 129], BF16) for _ in range(NB)],
            b=[Buf() for _ in range(NB)])
    vb = [P.sbuf([128, 129], BF16) for _ in range(4)]
    bvb = [Buf() for _ in range(4)]
    stm = [P.sbuf([128, 128], BF16) for _ in range(4)]
    bstm = [Buf() for _ in range(4)]
    chat = [P.sbuf([128, 129], BF16) for _ in range(4)]
    bchat = [Buf() for _ in range(4)]
    den = [P.sbuf([128, 1], F32) for _ in range(4)]
    bden = [Buf() for _ in range(4)]
    den2 = [P.sbuf([128, 1], F32) for _ in range(4)]
    bden2 = [Buf() for _ in range(4)]
    hB = [P.sbuf([128, 512], F32) for _ in range(2)]
    bhB = [Buf() for _ in range(2)]
    sos = [P.sbuf([128, 512], BF16) for _ in range(2)]
    bsos = [Buf() for _ in range(2)]
    gso = P.sbuf([128, 512], F32)
    bgso = Buf()
    hs = P.sbuf([128, 512], F32)
    bhs = Buf()
    junk3 = P.sbuf([128, 128], F32)
    bj3 = Buf()
    ss3 = P.sbuf([128, 4], F32)
    bss3 = Buf()
    memt = P.sbuf([128, 512], BF16)
    bmemt = Buf()
    FB4 = 4 if NOWN % 4 == 0 else 1
    memT = [P.sbuf([128, 4, 128 * FB4], BF16) for _ in range(2)]
    bmemT = [Buf() for _ in range(2)]
    PROT = [0, 1, 2, 3, 4]
    cnt = {"A": 0, "B": 0, "x": 0, "f": 0}

    def step(d_, c, output):
        betaT, clmT, decT, NCd = (betaA, clmA, decA, NCA) if d_ == "A" else (betaB, clmB, decB_, NCB)
        bT = bTA if d_ == "A" else bTB
        mask = maskA if d_ == "A" else maskB
        L = ld[d_]
        s = cnt[d_] % NB
        cnt[d_] += 1
        bl = L["b"][s]
        if output:
            DMA(P, L["q"][s][:], MQT[:, :, c * 128:(c + 1) * 128], (), [bl])
            DMA(P, L["k"][s][:], MKT[:, :, c * 128:(c + 1) * 128], (), [bl])
        DMA(P, L["kt"][s][:], MK[:, c, :], (), [bl])
        DMA(P, L["v"][s][:], MV[:, c, :], (), [bl])
        for h in range(4):
            u, bu = U[(d_, h)]
            w = cnt["x"] % 4
            cnt["x"] += 1
            TS(P, vb[w][:], L["v"][s][:, h * 129:(h + 1) * 129], betaT[:, h * NCd + c:h * NCd + c + 1], None, ALU.mult, reads=[bl, bT], writes=[bvb[w]])
            if output:
                p1 = rot(PROT)
                MM(P, BK[p1][:, 0:128], L["k"][s][:, h, :], L["q"][s][:, h, :], True, True, [bl], bk[p1])
                TT(P, stm[w][:], BK[p1][:, 0:128], mask[:], ALU.mult, [bmask], [bstm[w]], [bk[p1]])
                TS(P, chat[w][:], u[:], decT[:, h * NCd + c:h * NCd + c + 1], None, ALU.mult, reads=[bu, bT], writes=[bchat[w]])
                p2 = rot(PROT)
                MM(P, BK[p2][:, 0:129], stm[w][:], vb[w][:], True, False, [bstm[w], bvb[w]], bk[p2])
                MM(P, BK[p2][:, 0:129], L["q"][s][:, h, :], chat[w][:], False, True, [bl, bchat[w]], bk[p2])
                TS(P, den[w][:], BK[p2][:, 128:129], clmT[:, h * NCd + c:h * NCd + c + 1], None, ALU.max, reads=[bT], writes=[bden[w]], psum=[bk[p2]])
                TS(P, den2[w][:], BK[p2][:, 128:129], -1.0, clmT[:, h * NCd + c:h * NCd + c + 1], ALU.mult, ALU.max, reads=[bT], writes=[bden2[w]], psum=[bk[p2]])
                TT(P, den[w][:], den[w][:], den2[w][:], ALU.max, [bden[w], bden2[w]], [bden[w]])
                RECIP(P, den[w][:], den[w][:], [bden[w]], [bden[w]])
                if d_ == "A":
                    TS(P, hA[:, c, h * 128:(h + 1) * 128], BK[p2][:, 0:128], den[w][:], None, ALU.mult, reads=[bden[w]], writes=[bhA[c]], psum=[bk[p2]])
                else:
                    fs = cnt["f"] % 2
                    TS(P, hB[fs][:, h * 128:(h + 1) * 128], BK[p2][:, 0:128], den[w][:], None, ALU.mult, reads=[bden[w]], writes=[bhB[fs]], psum=[bk[p2]])
            p3 = rot(PROT)
            MM(P, BK[p3][:, 0:129], L["kt"][s][:, h * 128:(h + 1) * 128], vb[w][:], True, True, [bl, bvb[w]], bk[p3])
            TS(P, u[:], u[:], decT[:, h * NCd + c:h * NCd + c + 1], None, ALU.mult, reads=[bu, bT], writes=[bu])
            TT(P, u[:], u[:], BK[p3][:, 0:129], ALU.add, [bu], [bu], [bk[p3]])

    def finalize(c):
        fs = cnt["f"] % 2
        cnt["f"] += 1
        DMA(P, sos[fs][:], SO[:, c, :], (), [bsos[fs]])
        TT(P, hs[:], hA[:, c, :], hB[fs][:], ALU.add, [bhA[c], bhB[fs]], [bhs])
        for h in range(4):
            ACT(P, junk3[:], hs[:, h * 128:(h + 1) * 128], AF.Square, [bhs], [bj3, bss3], accum_out=ss3[:, h:h + 1])
        ACT(P, ss3[:], ss3[:], AF.Sqrt, [bss3, b_const], [bss3], bias=epst[:], scale=1.0 / 128)
        RECIP(P, ss3[:], ss3[:], [bss3], [bss3])
        TT(P, gso[:], ogb[:], sos[fs][:], ALU.mult, [bog, bsos[fs]], [bgso], eng="pool")
        TT(P, hs[:].rearrange("p (h e) -> p h e", e=128), hs[:].rearrange("p (h e) -> p h e", e=128),
           ss3[:, :, None].to_broadcast([128, 4, 128]), ALU.mult, [bhs, bss3], [bhs])
        TT(P, memt[:], hs[:], gso[:], ALU.mult, [bhs, bgso], [bmemt])
        tb = rot([5, 6])
        ptv = BK[tb][:].bitcast(BF16)
        for h in range(4):
            TR(P, ptv[:, h * 128:(h + 1) * 128], memt[:, h * 128:(h + 1) * 128], identb[:], [bmemt, b_const], bk[tb])
        fg = (c // FB4) % 2
        fsub = c % FB4
        CP(P, memT[fg][:, :, fsub * 128:(fsub + 1) * 128], ptv[:, 0:512].rearrange("p (h t) -> p h t", t=128), (), [bmemT[fg]], [bk[tb]], eng="act")
        if fsub == 0:
            DMA(P, MIXT[:, 4:8, c * 128:(c + FB4) * 128], memT[fg][:], [bmemT[fg]], [b_mixt], q="pool")

    for k in range(NOWN):
        step("A", k, True)
        step("B", NTT - 1 - k, False)
    for k in range(NOWN):
        c = NOWN - 1 - k
        step("B", c, True)
        finalize(c)
    P.end()

    P.begin()
    wo = P.sbuf([128, NCH, D], BF16)
    bwo = Buf()
    wst4 = [P.sbuf([128, D], F32) for _ in range(2)]
    bws4 = [Buf() for _ in range(2)]
    for c in range(NCH):
        s = c % 2
        DMA(P, wst4[s][:], w_out[c * 128:(c + 1) * 128, :], (), [bws4[s]])
        CP(P, wo[:, c, :], wst4[s][:], [bws4[s]], [bwo], eng="act" if c % 2 else "dve")
    g2T = P.sbuf([128, NCH], F32)
    bg2 = Buf()
    load_colvec(n2g, NCH, g2T[:], bg2, 7)
    MB4 = 4 if NOWN % 4 == 0 else 1
    mx_ = [P.sbuf([128, NCH, 128 * MB4], BF16) for _ in range(2)]
    bmx = [Buf() for _ in range(2)]
    x4 = [P.sbuf([128, D], F32) for _ in range(2)]
    bx4 = [Buf() for _ in range(2)]
    h4 = [P.sbuf([128, D], F32) for _ in range(2)]
    bh4 = [Buf() for _ in range(2)]
    junk4 = P.sbuf([128, D], BF16)
    bj4 = Buf()
    ss4 = [P.sbuf([128, 1], F32) for _ in range(2)]
    bss4 = [Buf() for _ in range(2)]
    hs4 = [P.sbuf([128, D], BF16) for _ in range(2)]
    bhs4 = [Buf() for _ in range(2)]
    xn2 = [P.sbuf([128, NCH, 128 * MB4], BF16) for _ in range(2)]
    bxn2 = [Buf() for _ in range(2)]
    b_y = Buf()
    b_xn2t = Buf()
    for i in range(NOWN):
        s = i % 2
        mg = (i // MB4) % 2
        msub = i % MB4
        if msub == 0:
            DMA(P, mx_[mg][:], MIXT[:, :, i * 128:(i + MB4) * 128], [b_mixt], [bmx[mg]])
        DMA(P, x4[s][:], x[i * 128:(i + 1) * 128, :], (), [bx4[s]])
        for hh in range(2):
            pb_ = rot([0, 1, 2, 3])
            for c in range(NCH):
                MM(P, BK[pb_][:, 0:512], mx_[mg][:, c, msub * 128:(msub + 1) * 128], wo[:, c, hh * 512:(hh + 1) * 512], c == 0, c == NCH - 1,
                   [bmx[mg], bwo], bk[pb_])
            TT(P, h4[s][:, hh * 512:(hh + 1) * 512], BK[pb_][:, 0:512], x4[s][:, hh * 512:(hh + 1) * 512], ALU.add, [bx4[s]], [bh4[s]], [bk[pb_]])
        DMA(P, y[i * 128:(i + 1) * 128, :], h4[s][:], [bh4[s]], [b_y], q="pool")
        ACT(P, junk4[:], h4[s][:], AF.Square, [bh4[s]], [bj4, bss4[s]], accum_out=ss4[s][:])
        ACT(P, ss4[s][:], ss4[s][:], AF.Sqrt, [bss4[s], b_const], [bss4[s]], bias=epst[:], scale=1.0 / D)
        RECIP(P, ss4[s][:], ss4[s][:], [bss4[s]], [bss4[s]])
        ACT(P, hs4[s][:], h4[s][:], AF.Copy, [bh4[s], bss4[s]], [bhs4[s]], scale=ss4[s][:])
        tb = rot([4, 5, 6])
        pv = BK[tb][:].bitcast(BF16).rearrange("p (c t) -> p c t", c=NCH)
        for c in range(NCH):
            TR(P, pv[:, c, :], hs4[s][:, c * 128:(c + 1) * 128], identb[:], [bhs4[s], b_const], bk[tb])
        TT(P, xn2[mg][:, :, msub * 128:(msub + 1) * 128], pv, g2T[:, :, None].to_broadcast([128, NCH, 128]), ALU.mult, [bg2], [bxn2[mg]], [bk[tb]])
        if msub == MB4 - 1:
            DMA(P, XN2T[:, :, (i - MB4 + 1) * 128:(i + 1) * 128], xn2[mg][:], [bxn2[mg]], [b_xn2t], q="pool")
    P.end()

    build_peer(P, nc, dict(SH=SH, down=down, up=up, DT=DT, UB=UB, wq=wq, subk=subk, XN2T=XN2T, y=y, identb=identb, identf=identf,
                           iotab=iotab, iotaf=iotaf, b_const=b_const, rot=rot, kscr=kscr))
    P.finish()
    return nc


def build_peer(P, nc, L):
    SH = L["SH"]
    down, up, DT, UB, wq, subk, XN2T, y = L["down"], L["up"], L["DT"], L["UB"], L["wq"], L["subk"], L["XN2T"], L["y"]
    identb, identf, iotaf, b_const, rot = L["identb"], L["identf"], L["iotaf"], L["b_const"], L["rot"]
    BK, bk = P.banks, P.bk
    NOWN = SH // 128
    kscr = L["kscr"]
    ISEL = nc.dram_tensor("ISEL", [128, SH], F32, kind=kscr).ap()
    JSEL = nc.dram_tensor("JSEL", [128, SH], F32, kind=kscr).ap()
    GATE = nc.dram_tensor("GATE", [128, SH], F32, kind=kscr).ap()

    P.begin()
    wqb = P.sbuf([128, 8, 2048], BF16)
    bwq = Buf()
    wqs = [P.sbuf([128, 2048], F32) for _ in range(2)]
    bwqs = [Buf() for _ in range(2)]
    for c in range(8):
        s = c % 2
        DMA(P, wqs[s][:], wq[c * 128:(c + 1) * 128, :], (), [bwqs[s]])
        CP(P, wqb[:, c, :], wqs[s][:], [bwqs[s]], [bwq], eng="act" if c % 2 else "dve")
    sks = P.sbuf([128, 16, 128], F32)
    bsk = Buf()
    DMA(P, sks[:], subk.rearrange("a n d -> n a d"), (), [bsk])
    skb = P.sbuf([128, 16, 128], BF16)
    CP(P, skb[:], sks[:], [bsk], [bsk])
    subkT = P.sbuf([128, 16, 128], BF16)
    bskT = Buf()
    for half in range(2):
        b = rot([4, 5, 6, 7])
        pv = BK[b][:].bitcast(BF16)
        for a in range(8):
            TR(P, pv[:, a * 128:(a + 1) * 128], skb[:, half * 8 + a, :], identb[:], [bsk, b_const], bk[b])
        CP(P, subkT[:, half * 8:(half + 1) * 8, :].rearrange("p a n -> p (a n)"), pv, (), [bskT], [bk[b]])
    xs_ = [P.sbuf([128, 8, 128], BF16) for _ in range(2)]
    bxs_ = [Buf() for _ in range(2)]
    sel = [P.sbuf([128, 3, 128], F32) for _ in range(2)]
    bsel = [Buf() for _ in range(2)]
    selT = [P.sbuf([128, 3, 128], F32) for _ in range(2)]
    bselT = [Buf() for _ in range(2)]
    b_route = Buf()
    iota16 = iotaf[:, 0:16]

    class Slot:
        pass

    slots = []
    for _ in range(2):
        S_ = Slot()
        S_.qh = P.sbuf([128, 16, 128], BF16)
        S_.sc = P.sbuf([128, 16, 128], F32)
        S_.sc2 = P.sbuf([128, 16, 128], F32)
        S_.v = P.sbuf([128, 16, 16], F32)
        S_.ix = P.sbuf([128, 16, 16], U32)
        S_.ixf = P.sbuf([128, 16, 16], F32)
        S_.cand = P.sbuf([128, 8, 256], F32)
        S_.cand2 = P.sbuf([128, 8, 256], F32)
        S_.tv = P.sbuf([128, 8, 16], F32)
        S_.pos = P.sbuf([128, 8, 16], U32)
        S_.k12 = P.sbuf([128, 2, 128], U32)
        S_.k12f = P.sbuf([128, 2, 128], F32)
        S_.oh = [P.sbuf([128, 8, 16, 16], F32) for _ in range(2)]
        S_.ez = P.sbuf([128, 8], F32)
        S_.bqh, S_.btop, S_.btop2, S_.bcand, S_.bk12, S_.bez = Buf(), Buf(), Buf(), Buf(), Buf(), Buf()
        S_.boh = [Buf(), Buf()]
        S_.rowb1 = [[Buf() for _ in range(5)] for _ in range(16)]
        S_.rowb2 = [[Buf() for _ in range(5)] for _ in range(8)]
        slots.append(S_)

    def top16(src3, nrow, vals, idxs, scratch3, bsrc, brow):
        for r in range(nrow):
            P.op("dve", lambda e, o=vals[:, r, 0:8], i=src3[:, r, :]: e.max(out=o, in_=i), [bsrc], [brow[r][0]])
        yield
        for r in range(nrow):
            P.op("dve", lambda e, o=idxs[:, r, 0:8], m=vals[:, r, 0:8], i=src3[:, r, :]: e.max_index(out=o, in_max=m, in_values=i),
                 [bsrc, brow[r][0]], [brow[r][1]])
        yield
        for r in range(nrow):
            P.op("dve", lambda e, o=scratch3[:, r, :], m=vals[:, r, 0:8], i=src3[:, r, :]: e.match_replace(out=o, in_to_replace=m, in_values=i, imm_value=-1e30),
                 [bsrc, brow[r][0]], [brow[r][2]])
        yield
        for r in range(nrow):
            P.op("dve", lambda e, o=vals[:, r, 8:16], i=scratch3[:, r, :]: e.max(out=o, in_=i), [brow[r][2]], [brow[r][3]])
        yield
        for r in range(nrow):
            P.op("dve", lambda e, o=idxs[:, r, 8:16], m=vals[:, r, 8:16], i=scratch3[:, r, :]: e.max_index(out=o, in_max=m, in_values=i),
                 [brow[r][2], brow[r][3]], [brow[r][4]])
        yield

    def route(su):
        s = su % 2
        Q = slots[s]
        DMA(P, xs_[s][:], XN2T[:, :, su * 128:(su + 1) * 128], (), [bxs_[s]])
        for hp in range(16):
            b = rot([4, 5, 6, 7])
            for c in range(8):
                MM(P, BK[b][:, 0:128], wqb[:, c, hp * 128:(hp + 1) * 128], xs_[s][:, c, :], c == 0, c == 7, [bwq, bxs_[s]], bk[b])
            CP(P, Q.qh[:, hp, :], BK[b][:, 0:128], (), [Q.bqh], [bk[b]], eng="act")
        yield
        for q4 in range(4):
            b = rot([0, 1, 2, 3])
            for a in range(4):
                hp = q4 * 4 + a
                MM(P, BK[b][:, a * 128:(a + 1) * 128], Q.qh[:, hp, :], subkT[:, hp, :], True, True, [Q.bqh, bskT], bk[b], skip=True)
            CP(P, Q.sc[:, q4 * 4:(q4 + 1) * 4, :].rearrange("p a n -> p (a n)"), BK[b][:, 0:512], (), [Q.btop], [bk[b]], eng="act")
        yield
        yield from top16(Q.sc, 16, Q.v, Q.ix, Q.sc2, Q.btop, Q.rowb1)
        all1 = [b_ for r in range(16) for b_ in Q.rowb1[r]]
        CP(P, Q.ixf[:], Q.ix[:], all1, [Q.btop2])
        v4 = Q.v[:].rearrange("p (h two) k -> p h two k", two=2)
        TT(P, Q.cand[:].rearrange("p h (a b) -> p h a b", b=16), v4[:, :, 0, :, None].to_broadcast([128, 8, 16, 16]),
           v4[:, :, 1, None, :].to_broadcast([128, 8, 16, 16]), ALU.add, all1, [Q.bcand])
        yield
        yield from top16(Q.cand, 8, Q.tv, Q.pos, Q.cand2, Q.bcand, Q.rowb2)
        all2 = [b_ for r in range(8) for b_ in Q.rowb2[r]]
        P.op("dve", lambda e: e.tensor_single_scalar(out=Q.k12[:, 0, :], in_=Q.pos[:].rearrange("p h k -> p (h k)"), scalar=4, op=ALU.logical_shift_right), all2, [Q.bk12])
        P.op("dve", lambda e: e.tensor_single_scalar(out=Q.k12[:, 1, :], in_=Q.pos[:].rearrange("p h k -> p (h k)"), scalar=15, op=ALU.bitwise_and), all2, [Q.bk12])
        yield
        CP(P, Q.k12f[:], Q.k12[:], [Q.bk12], [Q.bk12])
        g3 = sel[s][:, 2, :].rearrange("p (h k) -> p h k", k=16)
        TT(P, g3, Q.tv[:], Q.tv[:, :, 0:1].to_broadcast([128, 8, 16]), ALU.subtract, all2, [bsel[s]])
        ACT(P, sel[s][:, 2, :], sel[s][:, 2, :], AF.Exp, [bsel[s]], [bsel[s]])
        yield
        ix4 = Q.ixf[:].rearrange("p (h two) k -> p h two k", two=2)
        for w_ in range(2):
            kf = Q.k12f[:, w_, :].rearrange("p (h k) -> p h k", k=16)
            TT(P, Q.oh[w_][:], kf[:, :, :, None].to_broadcast([128, 8, 16, 16]), iota16[:, None, None, :].to_broadcast([128, 8, 16, 16]), ALU.is_equal,
               [Q.bk12, b_const], [Q.boh[w_]])
            TT(P, Q.oh[w_][:], Q.oh[w_][:], ix4[:, :, w_, None, :].to_broadcast([128, 8, 16, 16]), ALU.mult, [Q.boh[w_], Q.btop2], [Q.boh[w_]], eng="pool")
            yield
        RED(P, Q.ez[:], g3, ALU.add, [bsel[s]], [Q.bez])
        RECIP(P, Q.ez[:], Q.ez[:], [Q.bez], [Q.bez])
        TT(P, g3, g3, Q.ez[:, :, None].to_broadcast([128, 8, 16]), ALU.mult, [bsel[s], Q.bez], [bsel[s]])
        yield
        for w_ in range(2):
            RED(P, sel[s][:, w_, :], Q.oh[w_][:].rearrange("p h k kk -> p (h k) kk"), ALU.add, [Q.boh[w_]], [bsel[s]])
        yield
        b = rot([4, 5, 6, 7])
        for w_ in range(3):
            TR(P, BK[b][:, w_ * 128:(w_ + 1) * 128], sel[s][:, w_, :], identf[:], [bsel[s], b_const], bk[b])
        CP(P, selT[s][:].rearrange("p a t -> p (a t)"), BK[b][:, 0:384], (), [bselT[s]], [bk[b]], eng="act")
        DMA(P, ISEL[:, su * 128:(su + 1) * 128], selT[s][:, 0, :], [bselT[s]], [b_route], q="pool")
        DMA(P, JSEL[:, su * 128:(su + 1) * 128], selT[s][:, 1, :], [bselT[s]], [b_route], q="pool")
        DMA(P, GATE[:, su * 128:(su + 1) * 128], selT[s][:, 2, :], [bselT[s]], [b_route], q="pool")

    active = []
    nxt = 0
    while nxt < NOWN or active:
        while len(active) < 2 and nxt < NOWN:
            if active and nxt % 2 == active[0][0] % 2:
                break
            active.append((nxt, route(nxt)))
            nxt += 1
        for item in list(active):
            try:
                next(item[1])
            except StopIteration:
                active.remove(item)
    P.end()

    P.begin()
    TG = 256 if SH >= 256 else SH
    NTS = TG // 128
    NG = SH // TG
    TB = 16
    NBT = TG // TB
    IB = 2
    NTB = 4
    xg = [P.sbuf([128, 8, TG], BF16) for _ in range(2)]
    bxg = [Buf() for _ in range(2)]
    st_ = [P.sbuf([128, 3, TG], F32) for _ in range(2)]
    bst = [Buf() for _ in range(2)]
    Aoh = [P.sbuf([128, TB, 128], BF16) for _ in range(2)]
    bA = [Buf() for _ in range(2)]
    Boh = [P.sbuf([128, TB, 128], BF16) for _ in range(2)]
    bB = [Buf() for _ in range(2)]
    Gall = [P.sbuf([128, TG, 128], BF16) for _ in range(2)]
    bG = [Buf() for _ in range(2)]
    dtb = [P.sbuf([128, IB, 1024], BF16) for _ in range(NTB)]
    ubb = [P.sbuf([128, IB, 1024], BF16) for _ in range(NTB)]
    bdt = [Buf() for _ in range(NTB)]
    bub = [Buf() for _ in range(NTB)]
    LA = 4
    NGB = LA + 2
    ge = [P.sbuf([128, TG], BF16) for _ in range(NGB)]
    bge = [Buf() for _ in range(NGB)]
    Wt = [P.sbuf([128, TG], BF16) for _ in range(NGB)]
    bW = [Buf() for _ in range(NGB)]
    hrow = [P.sbuf([128, 512], F32) for _ in range(4)]
    bhr = [Buf() for _ in range(4)]
    b_yout = Buf()
    cn = {"A": 0, "G": 0, "h": 0}
    abank = {}
    iotab = L["iotab"]

    def load_group(tg):
        gs = tg % 2
        t_lo = tg * TG
        DMA(P, xg[gs][:], XN2T[:, :, t_lo:t_lo + TG], (), [bxg[gs]])
        DMA(P, st_[gs][:, 0, :], ISEL[:, t_lo:t_lo + TG], (), [bst[gs]])
        DMA(P, st_[gs][:, 1, :], JSEL[:, t_lo:t_lo + TG], (), [bst[gs]])
        DMA(P, st_[gs][:, 2, :], GATE[:, t_lo:t_lo + TG], (), [bst[gs]])

    free_banks = [4, 5, 6, 7]

    def next_bank():
        assert free_banks, "PSUM ring exhausted"
        return free_banks.pop(0)

    def g_part(tg, bi, part):
        gs = tg % 2
        a_s = bi % 2
        hf = TB // 2
        half = part % 2
        t0_ = bi * TB + half * hf
        sl = slice(half * hf, (half + 1) * hf)
        io3 = iotaf[:, None, :].to_broadcast([128, hf, 128])
        if part < 2:
            TT(P, Aoh[a_s][:, sl, :], io3, st_[gs][:, 0, t0_:t0_ + hf, None].to_broadcast([128, hf, 128]), ALU.is_equal, [bst[gs], b_const], [bA[a_s]])
            TT(P, Aoh[a_s][:, sl, :], Aoh[a_s][:, sl, :], st_[gs][:, 2, t0_:t0_ + hf, None].to_broadcast([128, hf, 128]), ALU.mult, [bst[gs], bA[a_s]], [bA[a_s]])
        else:
            TT(P, Boh[a_s][:, sl, :], io3, st_[gs][:, 1, t0_:t0_ + hf, None].to_broadcast([128, hf, 128]), ALU.is_equal, [bst[gs], b_const], [bB[a_s]])

    def g_round(tg, bi, rd):
        gs = tg % 2
        a_s = bi % 2
        t4 = rd * 4
        b = next_bank()
        cn["G"] += 1
        for t in range(4):
            MM(P, BK[b][:, t * 128:(t + 1) * 128], Boh[a_s][:, t4 + t, :], Aoh[a_s][:, t4 + t, :], True, True, [bA[a_s], bB[a_s]], bk[b], skip=True)
        tt0 = bi * TB + t4
        CP(P, Gall[gs][:, tt0:tt0 + 4, :].rearrange("p t i -> p (t i)"), BK[b][:, 0:512], (), [bG[gs]], [bk[b]], eng="act")
        free_banks.append(b)

    def g_build(tg, bi):
        for part in range(4):
            g_part(tg, bi, part)

    def g_mm(tg, bi):
        for rd in range(TB // 4):
            g_round(tg, bi, rd)

    NBLK = 128 // IB

    def load_tab(tg, blk):
        ts_ = (tg * NBLK + blk) % NTB
        DMA(P, dtb[ts_][:], DT[:, blk * IB:(blk + 1) * IB, :], (), [bdt[ts_]])
        DMA(P, ubb[ts_][:], UB[:, blk * IB:(blk + 1) * IB, :], (), [bub[ts_]])

    def emit_aT(tg, i):
        gs = tg % 2
        ts_ = (tg * NBLK + i // IB) % NTB
        ab = next_bank()
        abank[(tg, i)] = ab
        for c in range(8):
            MM(P, BK[ab][:, 0:TG], dtb[ts_][:, i % IB, c * 128:(c + 1) * 128], xg[gs][:, c, :], c == 0, c == 7, [bdt[ts_], bxg[gs]], bk[ab])

    def emit_mid(tg, i):
        gs = tg % 2
        es = i % NGB
        ab = abank.pop((tg, i))
        ACT(P, ge[es][:], BK[ab][:, 0:TG], AF.Gelu, (), [bge[es]], [bk[ab]])
        free_banks.append(ab)
        TT(P, Wt[es][:], ge[es][:], Gall[gs][:, :, i], ALU.mult, [bge[es], bG[gs]], [bW[es]], eng="dve")

    def emit_up(tg, i):
        es = i % NGB
        ts_ = (tg * NBLK + i // IB) % NTB
        for ts2 in range(NTS):
            for dh in range(2):
                ob = ts2 * 2 + dh
                MM(P, BK[ob][:, 0:512], Wt[es][:, ts2 * 128:(ts2 + 1) * 128], ubb[ts_][:, i % IB, dh * 512:(dh + 1) * 512], i == 0, i == 127,
                   [bW[es], bub[ts_]], bk[ob])

    load_group(0)
    for bi in range(NBT):
        g_build(0, bi)
        g_mm(0, bi)
    for tg in range(NG):
        gs = tg % 2
        t_lo = tg * TG
        if tg + 1 < NG:
            load_group(tg + 1)
        if tg == 0:
            for blk in range(min(NTB - 1, NBLK)):
                load_tab(tg, blk)
        for k in range(LA):
            emit_aT(tg, k)
            emit_mid(tg, k)
        for i in range(128):
            if i % IB == 0 and i // IB + NTB - 1 < NBLK:
                load_tab(tg, i // IB + NTB - 1)
            emit_up(tg, i)
            if i + LA < 128:
                emit_aT(tg, i + LA)
                emit_mid(tg, i + LA)
            if tg + 1 < NG:
                IV = 128 // NBT
                bi_ = i // IV
                ph = i % IV
                if IV >= 8:
                    if ph % 2 == 0:
                        g_part(tg + 1, bi_, ph // 2)
                    elif bi_ >= 1:
                        g_round(tg + 1, bi_ - 1, ph // 2)
                elif ph == IV - 1:
                    if bi_ >= 1:
                        g_mm(tg + 1, bi_ - 1)
                    g_build(tg + 1, bi_)
                if i == 127:
                    g_mm(tg + 1, NBT - 1)
        if tg + 1 < NG:
            for blk in range(min(NTB - 1, NBLK)):
                load_tab(tg + 1, blk)
        for ts2 in range(NTS):
            r0 = t_lo + ts2 * 128
            for dh in range(2):
                hs_ = cn["h"] % 4
                cn["h"] += 1
                ob = ts2 * 2 + dh
                DMA(P, hrow[hs_][:], y[r0:r0 + 128, dh * 512:(dh + 1) * 512], (), [bhr[hs_]])
                TT(P, hrow[hs_][:], hrow[hs_][:], BK[ob][:, 0:512], ALU.add, [bhr[hs_]], [bhr[hs_]], [bk[ob]])
                DMA(P, y[r0:r0 + 128, dh * 512:(dh + 1) * 512], hrow[hs_][:], [bhr[hs_]], [b_yout], q="pool")
    P.end()


_NC_CACHE = {}


def rope_tables(pos):
    pos = np.asarray(pos)
    row = (pos // 64).astype(np.float32)
    col = (pos % 64).astype(np.float32)
    inv = (10000.0 ** (-np.arange(0, 32, 2, dtype=np.float32) / 32)).astype(np.float32)
    ang = np.concatenate([row[:, None] * inv, col[:, None] * inv], axis=-1).astype(np.float32)
    return np.cos(ang).astype(np.float32), np.sin(ang).astype(np.float32)


def make_in_maps(inp, debug=False):
    x = np.asarray(inp["x"], np.float32)
    B, S, _ = x.shape
    SH = S // 2
    f = lambda k: np.asarray(inp[k], np.float32)[0]
    w_in = f("w_in")
    perm = np.arange(2832)
    perm_flip = perm.copy()
    for base in (2816, 2824):
        perm_flip[base:base + 4] = np.arange(base + 4, base + 8)
        perm_flip[base + 4:base + 8] = np.arange(base, base + 4)
    convw = f("ml_conv_w")
    ibias = f("ml_igate_bias")
    fbias = f("ml_fgate_bias")
    common = {
        "n1g": f("norm1_gain").reshape(8, 128), "n2g": f("norm2_gain").reshape(8, 128),
        "qg": f("att_q_gain"), "kg": f("att_k_gain"), "convb": f("ml_conv_b").reshape(8, 128),
        "og": f("ml_out_gain").reshape(512), "w_out": f("w_out"), "wq": f("peer_w_query"),
        "subk": f("peer_sub_keys").reshape(16, 128, 128), "down": f("peer_down"), "up": f("peer_up"),
    }
    maps = []
    for c in range(2 * B):
        b, hf = c // 2, c % 2
        m = dict(common)
        if hf == 0:
            m["x"] = np.ascontiguousarray(x[b])
            pos = np.arange(S)
            m["w_in"] = w_in
            m["convw"] = convw
            m["ibias"] = np.ascontiguousarray(ibias.reshape(8, 1))
            m["fbias"] = np.ascontiguousarray(fbias.reshape(8, 1))
        else:
            m["x"] = np.ascontiguousarray(x[b, ::-1])
            pos = np.arange(S)[::-1]
            m["w_in"] = np.ascontiguousarray(w_in[:, perm_flip])
            m["convw"] = np.ascontiguousarray(convw[::-1])
            m["ibias"] = np.ascontiguousarray(ibias[::-1].reshape(8, 1))
            m["fbias"] = np.ascontiguousarray(fbias[::-1].reshape(8, 1))
        cs, sn = rope_tables(pos)
        m["cos"], m["sin"] = cs, sn
        maps.append(m)
    return maps, SH


def kernel(**inp):
    maps, SH = make_in_maps(inp)
    if SH not in _NC_CACHE:
        _NC_CACHE[SH] = build(SH)
    nc = _NC_CACHE[SH]
    res = run_bass_kernel_spmd(nc, maps, core_ids=list(range(len(maps))))
    B = len(maps) // 2
    out = np.zeros((B, 2 * SH, D), np.float32)
    for c in range(2 * B):
        b, hf = c // 2, c % 2
        yc = np.asarray(res.results[c]["y"], np.float32)
        if hf == 0:
            out[b, :SH] = yc
        else:
            out[b, SH:] = yc[::-1]
    return out
```

```python
import contextlib
import numpy as np
import ml_dtypes
import concourse.bass as bass
import concourse.mybir as mybir
from concourse.bass_utils import run_bass_kernel_spmd

F32 = mybir.dt.float32
BF16 = mybir.dt.bfloat16
I32 = mybir.dt.int32
U32 = mybir.dt.uint32
ALU = mybir.AluOpType
AF = mybir.ActivationFunctionType
AX = mybir.AxisListType

EPOCH = 12000
N_CH = 24
N_CH_SP = 16
ENGS = ("pe", "act", "dve", "pool", "sp")


class Buf:
    __slots__ = ("w", "r")

    def __init__(self):
        self.w = None
        self.r = []


class PBuf:
    __slots__ = ("last",)

    def __init__(self):
        self.last = {}


class Op:
    __slots__ = ("eng", "fn", "deps", "dma", "idx", "sig", "semi", "semv", "ch")


class Prog:
    def __init__(self, nc):
        self.nc = nc
        self.ops = []
        self.g = contextlib.ExitStack()
        self.p = None
        self.pstart = 0
        self.last_dma = [None] * N_CH
        self.rr = 0
        self.rr_pool = 0
        self.barrier = {}
        self.cnt = {e: 0 for e in ENGS}
        self.esem = {e: [] for e in ENGS}
        self.chsem = [self.g.enter_context(nc.semaphore(f"sd{c}")) for c in range(N_CH)]
        self.chcnt = [0] * N_CH
        self.waited = {e: {} for e in ENGS}
        self.last_eng = {e: None for e in ENGS}
        self.big = self.g.enter_context(nc.psum_tensor("psbig", [128, 4096], F32))
        self.banks = [self.big[:, k * 512:(k + 1) * 512] for k in range(8)]
        self.bk = [PBuf() for _ in range(8)]
        self.nm = 0

    def begin(self):
        self.p = contextlib.ExitStack()

    def sbuf(self, shape, dt, name=None):
        self.nm += 1
        return self.p.enter_context(self.nc.sbuf_tensor(name or f"t{self.nm}", list(shape), dt))

    def gsbuf(self, shape, dt, name=None):
        self.nm += 1
        return self.g.enter_context(self.nc.sbuf_tensor(name or f"g{self.nm}", list(shape), dt))

    def op(self, eng, fn, reads=(), writes=(), psum=(), dma=False):
        o = Op()
        o.eng, o.fn, o.dma = eng, fn, dma
        o.sig, o.semi, o.semv, o.ch = False, None, None, None
        o.idx = len(self.ops)
        deps = set()
        for b in reads:
            if b.w is not None:
                deps.add(b.w)
        for b in writes:
            if b.w is not None:
                deps.add(b.w)
            deps.update(b.r)
        for pb in psum:
            for e2, i2 in pb.last.items():
                if e2 != eng:
                    deps.add(i2)
        if dma:
            if eng == "pool":
                ch = N_CH_SP + self.rr_pool % (N_CH - N_CH_SP)
                self.rr_pool += 1
            else:
                ch = self.rr % N_CH_SP
                self.rr += 1
            o.ch = ch
            if self.last_dma[ch] is not None:
                deps.add(self.last_dma[ch])
            self.last_dma[ch] = o.idx
        if eng in self.barrier:
            deps.update(self.barrier.pop(eng))
        if eng == "pe":
            deps = {d for d in deps if self.ops[d].dma or self.ops[d].eng != "pe"}
        o.deps = deps
        for b in reads:
            b.r.append(o.idx)
        for b in writes:
            b.w = o.idx
            b.r = []
        for pb in psum:
            pb.last[eng] = o.idx
        self.ops.append(o)
        self.last_eng[eng] = o.idx
        return o

    def end(self):
        nc, ops = self.nc, self.ops
        lo = self.pstart
        new = ops[lo:]
        bar = set()
        for e in ENGS:
            if self.last_eng[e] is not None:
                bar.add(self.last_eng[e])
        for c in range(N_CH):
            if self.last_dma[c] is not None:
                bar.add(self.last_dma[c])
        for o in new:
            best, keep = {}, set()
            for d in o.deps:
                od = ops[d]
                if od.dma:
                    keep.add(d)
                elif od.eng not in best or best[od.eng] < d:
                    best[od.eng] = d
            keep.update(best.values())
            o.deps = keep
            for d in keep:
                if d >= lo:
                    ops[d].sig = True
        for d in bar:
            if d >= lo:
                ops[d].sig = True
        for o in new:
            if o.dma:
                self.chcnt[o.ch] += 16
                o.semv = self.chcnt[o.ch]
            elif o.sig:
                self.cnt[o.eng] += 1
                ep = (self.cnt[o.eng] - 1) // EPOCH
                o.semi = ep
                o.semv = self.cnt[o.eng] - ep * EPOCH
                while len(self.esem[o.eng]) <= ep:
                    self.esem[o.eng].append(self.g.enter_context(nc.semaphore(f"s{o.eng}{len(self.esem[o.eng])}")))
        engobj = {"pe": "tensor", "act": "scalar", "dve": "vector", "pool": "gpsimd", "sp": "sync"}
        per = {e: [o for o in new if o.eng == e] for e in ENGS}

        def run(ename, eng):
            waited = self.waited[ename]
            for o in per[ename]:
                for d in sorted(o.deps):
                    od = ops[d]
                    if od.dma:
                        key, sem = ("ch", od.ch), self.chsem[od.ch]
                    else:
                        if od.semv is None:
                            continue
                        key, sem = (od.eng, od.semi), self.esem[od.eng][od.semi]
                    if waited.get(key, 0) >= od.semv:
                        continue
                    waited[key] = od.semv
                    eng.wait_ge(sem, od.semv)
                ins = o.fn(eng)
                if o.dma:
                    ins.then_inc(self.chsem[o.ch], 16)
                elif o.sig:
                    ins.then_inc(self.esem[ename][o.semi], 1)

        with nc.Block() as block:
            for ename in ENGS:
                getattr(block, engobj[ename])(lambda e, n=ename: run(n, e))
        self.p.close()
        self.p = None
        self.pstart = len(ops)
        self.barrier = {e: set(bar) for e in ENGS}

    def finish(self):
        nc = self.nc

        def fin(eng):
            for c in range(N_CH):
                if self.chcnt[c] > 0:
                    eng.wait_ge(self.chsem[c], self.chcnt[c])
        with nc.Block() as block:
            block.sync(fin)
        self.g.close()


def DMA(P, out, in_, reads=(), writes=(), q="sp", slow=False):
    if slow:
        return P.op(q, lambda e, o=out, i=in_: e.dma_start(out=o, in_=i, allow_slow_non_contiguous=True), reads, writes, dma=True)
    return P.op(q, lambda e, o=out, i=in_: e.dma_start(out=o, in_=i), reads, writes, dma=True)


def MM(P, out, lhsT, rhs, start, stop, reads, pb, skip=False):
    if skip:
        return P.op("pe", lambda e: e.matmul(out, lhsT=lhsT, rhs=rhs, start=start, stop=stop, skip_group_check=True), reads, (), [pb])
    return P.op("pe", lambda e: e.matmul(out, lhsT=lhsT, rhs=rhs, start=start, stop=stop), reads, (), [pb])


def TR(P, out, in_, ident, reads, pb):
    return P.op("pe", lambda e: e.transpose(out=out, in_=in_, identity=ident), reads, (), [pb])


def ACT(P, out, in_, func, reads=(), writes=(), psum=(), **kw):
    return P.op("act", lambda e: e.activation(out=out, in_=in_, func=func, **kw), reads, writes, psum)


def TT(P, out, in0, in1, op, reads=(), writes=(), psum=(), eng="dve"):
    return P.op(eng, lambda e: e.tensor_tensor(out=out, in0=in0, in1=in1, op=op), reads, writes, psum)


def TS(P, out, in0, s1, s2, op0, op1=None, reads=(), writes=(), psum=(), eng="dve"):
    if op1 is None:
        return P.op(eng, lambda e: e.tensor_scalar(out=out, in0=in0, scalar1=s1, scalar2=None, op0=op0), reads, writes, psum)
    return P.op(eng, lambda e: e.tensor_scalar(out=out, in0=in0, scalar1=s1, scalar2=s2, op0=op0, op1=op1), reads, writes, psum)


def STT(P, out, in0, scalar, in1, op0, op1, reads=(), writes=(), psum=(), eng="dve"):
    return P.op(eng, lambda e: e.scalar_tensor_tensor(out=out, in0=in0, scalar=scalar, in1=in1, op0=op0, op1=op1),
                reads, writes, psum)


def CP(P, out, in_, reads=(), writes=(), psum=(), eng="dve"):
    if eng == "act":
        return P.op("act", lambda e: e.copy(out=out, in_=in_), reads, writes, psum)
    return P.op(eng, lambda e: e.tensor_copy(out=out, in_=in_), reads, writes, psum)


def MSET(P, ap, val, writes=(), eng="dve"):
    return P.op(eng, lambda e: e.memset(ap, val), (), writes)


def RED(P, out, in_, op, reads=(), writes=(), psum=(), eng="dve"):
    return P.op(eng, lambda e: e.tensor_reduce(out=out, in_=in_, axis=AX.X, op=op), reads, writes, psum)


def RECIP(P, out, in_, reads=(), writes=()):
    return P.op("dve", lambda e: e.reciprocal(out=out, in_=in_), reads, writes)


D = 1024
NCH = 8
EPS = 1e-6
NE = 16384


def build(SH, debug=False):
    T = 2 * SH
    NTT = T // 128
    NOWN = SH // 128
    nc = bass.Bass("TRN2", target_bir_lowering=False)
    kin = "ExternalInput"
    kscr = "ExternalOutput" if debug else "Internal"

    def din(name, shape, dt=F32):
        return nc.dram_tensor(name, list(shape), dt, kind=kin).ap()

    def dscr(name, shape, dt):
        return nc.dram_tensor(name, list(shape), dt, kind=kscr).ap()

    x = din("x", [T, D])
    cosd = din("cos", [T, 32])
    sind = din("sin", [T, 32])
    w_in = din("w_in", [D, 2832])
    n1g = din("n1g", [NCH, 128])
    n2g = din("n2g", [NCH, 128])
    qg = din("qg", [64])
    kg = din("kg", [64])
    convw = din("convw", [3, 1024])
    convb = din("convb", [NCH, 128])
    ibias = din("ibias", [8, 1])
    fbias = din("fbias", [8, 1])
    og = din("og", [512])
    w_out = din("w_out", [D, D])
    wq = din("wq", [D, 2048])
    subk = din("subk", [16, 128, 128])
    down = din("down", [NE, D])
    up = din("up", [NE, D])
    y = nc.dram_tensor("y", [SH, D], F32, kind="ExternalOutput").ap()

    HNT = dscr("HNT", [NCH, 128, T + 2], BF16)
    QR = dscr("QR", [64, 8, SH], BF16)
    KR = dscr("KR", [64, 2, T], BF16)
    V1 = dscr("V1", [128, NTT, 130], BF16)
    MQT = dscr("MQT", [128, 4, SH], BF16)
    MKT = dscr("MKT", [128, 4, T], BF16)
    MK = dscr("MK", [128, NTT, 512], BF16)
    MV = dscr("MV", [128, NTT, 4 * 129], BF16)
    SO = dscr("SO", [128, NOWN, 512], BF16)
    GIA = dscr("GIA", [4, T], F32)
    GFA = dscr("GFA", [4, T], F32)
    GIB = dscr("GIB", [4, T], F32)
    GFB = dscr("GFB", [4, T], F32)
    MIXT = dscr("MIXT", [128, 8, SH], BF16)
    XN2T = dscr("XN2T", [128, 8, SH], BF16)
    DT = dscr("DT", [128, 128, 1024], BF16)
    UB = dscr("UB", [128, 128, 1024], BF16)

    P = Prog(nc)
    BK = P.banks
    bk = P.bk

    identf = P.gsbuf([128, 128], F32, "identf")
    identb = P.gsbuf([128, 128], BF16, "identb")
    iotaf = P.gsbuf([128, 128], F32, "iotaf")
    iotab = P.gsbuf([128, 128], BF16, "iotab")
    epst = P.gsbuf([128, 1], F32, "epst")
    b_const = Buf()

    P.begin()
    iot = P.sbuf([128, 128], F32)
    P.op("pool", lambda e: e.iota(iot[:], pattern=[[1, 128]], base=0, channel_multiplier=-1,
                                   allow_small_or_imprecise_dtypes=True), (), [b_const])
    P.op("dve", lambda e: e.tensor_single_scalar(out=identf[:], in_=iot[:], scalar=0.0, op=ALU.is_equal), [b_const], [b_const])
    CP(P, identb[:], identf[:], [b_const], [b_const])
    P.op("pool", lambda e: e.iota(iotaf[:], pattern=[[1, 128]], base=0, channel_multiplier=0,
                                   allow_small_or_imprecise_dtypes=True), (), [b_const])
    CP(P, iotab[:], iotaf[:], [b_const], [b_const])
    MSET(P, epst[:], EPS, [b_const])
    P.end()

    rot_state = {"i": 0}

    def rot(banks):
        k = banks[rot_state["i"] % len(banks)]
        rot_state["i"] += 1
        return k

    def load_colvec(dram_nx128, n, dst, bdst, bank):
        st = P.sbuf([n, 128], F32)
        bs = Buf()
        DMA(P, st[:], dram_nx128, (), [bs])
        TR(P, BK[bank][:, 0:n], st[:], identf[0:n, 0:n], [bs, b_const], bk[bank])
        CP(P, dst, BK[bank][:, 0:n], (), [bdst], [bk[bank]])

    P.begin()
    g1T = P.sbuf([128, NCH], F32)
    bg1 = Buf()
    load_colvec(n1g, NCH, g1T[:], bg1, 7)
    zt = P.sbuf([128, NCH, 1], BF16)
    bz = Buf()
    MSET(P, zt[:], 0.0, [bz])
    HNTv = HNT.rearrange("c p t -> p c t")
    b_hnt = Buf()
    DMA(P, HNTv[:, :, 0:1], zt[:], [bz], [b_hnt], q="pool", slow=True)
    DMA(P, HNTv[:, :, T + 1:T + 2], zt[:], [bz], [b_hnt], q="pool", slow=True)
    xt = [P.sbuf([128, D], F32) for _ in range(4)]
    bxt = [Buf() for _ in range(4)]
    junk = P.sbuf([128, D], BF16)
    bjunk = Buf()
    xs = [P.sbuf([128, D], BF16) for _ in range(2)]
    bxs = [Buf() for _ in range(2)]
    ss = [P.sbuf([128, 1], F32) for _ in range(2)]
    bss = [Buf() for _ in range(2)]
    HB = 4 if NTT % 4 == 0 else 1
    hnt = [P.sbuf([128, NCH, 128 * HB], BF16) for _ in range(2)]
    bhn = [Buf() for _ in range(2)]

    def norm_transpose(i, src_rows, gT, bgT, dst_dram, banks, store_q):
        s = i % 2
        s4 = i % 4
        DMA(P, xt[s4][:], src_rows, (), [bxt[s4]])
        ACT(P, junk[:], xt[s4][:], AF.Square, [bxt[s4]], [bjunk, bss[s]], accum_out=ss[s][:])
        ACT(P, ss[s][:], ss[s][:], AF.Sqrt, [bss[s], b_const], [bss[s]], bias=epst[:], scale=1.0 / D)
        RECIP(P, ss[s][:], ss[s][:], [bss[s]], [bss[s]])
        ACT(P, xs[s][:], xt[s4][:], AF.Copy, [bxt[s4], bss[s]], [bxs[s]], scale=ss[s][:])
        b = rot(banks)
        pv = BK[b][:].bitcast(BF16).rearrange("p (c t) -> p c t", c=NCH)
        for c in range(NCH):
            TR(P, pv[:, c, :], xs[s][:, c * 128:(c + 1) * 128], identb[:], [bxs[s], b_const], bk[b])
        hg = (i // HB) % 2
        sub = i % HB
        TT(P, hnt[hg][:, :, sub * 128:(sub + 1) * 128], pv, gT[:, :, None].to_broadcast([128, NCH, 128]), ALU.mult, [bgT], [bhn[hg]], [bk[b]])
        if sub == HB - 1:
            i0 = i - (HB - 1)
            DMA(P, HNTv[:, :, 1 + i0 * 128:1 + (i + 1) * 128], hnt[hg][:], [bhn[hg]], [b_hnt], q=store_q)

    C_AQ, C_AK, C_AV = 0, 512, 640
    C_MV, C_MO = 768, 1280
    C_MQ = 1792
    C_MK = 1792 + 1536
    C_GI = C_MK + 1536
    C_GF = C_GI + 8
    NCOL = C_GF + 8
    WBD = dscr("WBD", [128, NCH, NCOL], BF16)
    wbs = P.sbuf([128, NCOL], BF16)
    bwbs = Buf()
    b_wbd = Buf()
    cwb = P.sbuf([128, 3, 1024], F32)
    bcw = Buf()
    DMA(P, cwb[:].rearrange("p a b -> p (a b)"), convw.rearrange("a b -> (a b)").partition_broadcast(128), (), [bcw])
    wst = [P.sbuf([128, 2832], F32) for _ in range(2)]
    bws = [Buf() for _ in range(2)]
    def wprep(c):
        s = c % 2
        DMA(P, wst[s][:], w_in[c * 128:(c + 1) * 128, :], (), [bws[s]])
        CP(P, wbs[:, 0:768], wst[s][:, 0:768], [bws[s]], [bwbs], eng="act")
        CP(P, wbs[:, C_MV:C_MV + 1024], wst[s][:, 1792:2816], [bws[s]], [bwbs], eng="pool")
        CP(P, wbs[:, C_GI:C_GI + 16], wst[s][:, 2816:2832], [bws[s]], [bwbs], eng="pool")
        for tap in range(3):
            TT(P, wbs[:, C_MQ + tap * 512:C_MQ + (tap + 1) * 512], wst[s][:, 768:1280], cwb[:, tap, 0:512], ALU.mult,
               [bws[s], bcw], [bwbs])
            TT(P, wbs[:, C_MK + tap * 512:C_MK + (tap + 1) * 512], wst[s][:, 1280:1792], cwb[:, tap, 512:1024], ALU.mult,
               [bws[s], bcw], [bwbs], eng="pool")
        DMA(P, WBD[:, c, :], wbs[:], [bwbs], [b_wbd], q="pool")

    wdone = 0
    for i in range(NTT):
        while wdone < NCH and wdone * NTT <= i * NCH:
            wprep(wdone)
            wdone += 1
        norm_transpose(i, x[i * 128:(i + 1) * 128, :], g1T, bg1, HNTv[:, :, 1 + i * 128:1 + (i + 1) * 128], [0, 1, 2], "pool")
    while wdone < NCH:
        wprep(wdone)
        wdone += 1
    P.end()

    P.begin()
    wb = P.sbuf([128, NCH, NCOL], BF16)
    bwb = Buf()
    DMA(P, wb[:], WBD, [b_wbd], [bwb])
    cbT = P.sbuf([128, NCH], F32)
    bcb = Buf()
    load_colvec(convb, NCH, cbT[:], bcb, 7)
    ib = P.sbuf([8, 1], F32)
    fbn = P.sbuf([8, 1], F32)
    bgb = Buf()
    DMA(P, ib[:], ibias, (), [bgb])
    DMA(P, fbn[:], fbias, (), [bgb])
    TS(P, fbn[:], fbn[:], -1.0, None, ALU.mult, reads=[bgb], writes=[bgb])
    gq = P.sbuf([128, 64], F32)
    gk = P.sbuf([128, 64], F32)
    bgq = Buf()
    DMA(P, gq[:], qg.partition_broadcast(128), (), [bgq])
    DMA(P, gk[:], kg.partition_broadcast(128), (), [bgq])
    TS(P, gq[:], gq[:], 0.125, None, ALU.mult, reads=[bgq], writes=[bgq])

    TW = 512 if SH >= 512 else SH
    NSUB = TW // 128
    hn = [P.sbuf([128, NCH, TW + 2], BF16) for _ in range(2)]
    bhn1 = [Buf() for _ in range(2)]
    cs = [P.sbuf([128, 32], F32) for _ in range(2)]
    sn = [P.sbuf([128, 32], F32) for _ in range(2)]
    bcs = [Buf() for _ in range(2)]
    sq = [P.sbuf([128, 640], F32) for _ in range(2)]
    bsq = [Buf() for _ in range(2)]
    ssq = [P.sbuf([128, 10], F32) for _ in range(2)]
    bssq = [Buf() for _ in range(2)]
    qn = [P.sbuf([128, 640], F32) for _ in range(2)]
    bqn = [Buf() for _ in range(2)]
    ta = [P.sbuf([128, 10, 32], F32) for _ in range(2)]
    tb_ = [P.sbuf([128, 10, 32], F32) for _ in range(2)]
    bta = [Buf() for _ in range(2)]
    btb = [Buf() for _ in range(2)]
    qr = [P.sbuf([128, 640], BF16) for _ in range(2)]
    bqr = [Buf() for _ in range(2)]
    mhalf = P.sbuf([128, 10], F32)
    bmh = Buf()
    MSET(P, mhalf[:], -0.5, [bmh])
    qT = [P.sbuf([64, 8, TW], BF16) for _ in range(2)]
    bqT = [Buf() for _ in range(2)]
    kTt = [P.sbuf([64, 2, TW], BF16) for _ in range(2)]
    bkT = [Buf() for _ in range(2)]
    v1 = [P.sbuf([128, NSUB, 130], BF16) for _ in range(2)]
    bv1 = [Buf() for _ in range(2)]
    for s in range(2):
        MSET(P, v1[s][:], 1.0, [bv1[s]])
    mv = [P.sbuf([128, NSUB, 4 * 129], BF16) for _ in range(2)]
    bmv = [Buf() for _ in range(2)]
    for s in range(2):
        MSET(P, mv[s][:], 1.0, [bmv[s]])
    so = [P.sbuf([128, NSUB, 512], BF16) for _ in range(2)]
    bso = [Buf() for _ in range(2)]
    mqT = [P.sbuf([128, 4, TW], BF16) for _ in range(2)]
    bmq = [Buf() for _ in range(2)]
    mkf = P.sbuf([128, TW], F32)
    bmkf = Buf()
    mkT = [P.sbuf([128, 4, TW], BF16) for _ in range(2)]
    bmk = [Buf() for _ in range(2)]
    mkt = [P.sbuf([128, NSUB, 512], BF16) for _ in range(2)]
    bmkt = [Buf() for _ in range(2)]
    gi = [P.sbuf([8, TW], F32) for _ in range(2)]
    gf = [P.sbuf([8, TW], F32) for _ in range(2)]
    bgi = [Buf() for _ in range(2)]
    bgf = [Buf() for _ in range(2)]
    b_p1out = Buf()
    ROT1 = [0, 1, 2, 3, 4, 5, 6, 7]

    def qk_norm_rope(pbank, col0, nh, gain, out_cols, own_s, pr, o0):
        n = nh * 64
        src = BK[pbank][:, col0:col0 + n]
        sqv = sq[pr][:, o0:o0 + n]
        ssv = ssq[pr][:, o0 // 64:o0 // 64 + nh]
        ACT(P, sqv, src, AF.Square, (), [bsq[pr]], [bk[pbank]])
        RED(P, ssv, sqv.rearrange("p (h d) -> p h d", d=64), ALU.add, [bsq[pr]], [bssq[pr]])
        TS(P, ssv, ssv, 1.0 / 64, EPS, ALU.mult, ALU.add, reads=[bssq[pr]], writes=[bssq[pr]], eng="pool")
        TT(P, ssv, ssv, mhalf[:, 0:nh], ALU.pow, [bssq[pr], bmh], [bssq[pr]], eng="pool")
        qv = qn[pr][:, o0:o0 + n].rearrange("p (h d) -> p h d", d=64)
        TT(P, qv, src.rearrange("p (h d) -> p h d", d=64), ssv[:, :, None].to_broadcast([128, nh, 64]), ALU.mult,
           [bssq[pr]], [bqn[pr]], [bk[pbank]])
        TT(P, qv, qv, gain[:, None, :].to_broadcast([128, nh, 64]), ALU.mult, [bqn[pr], bgq], [bqn[pr]])
        q4 = qn[pr][:, o0:o0 + n].rearrange("p (h i two) -> p h i two", two=2, i=32)
        t0, t1 = q4[:, :, :, 0], q4[:, :, :, 1]
        cb = cs[own_s][:, 0:32][:, None, :].to_broadcast([128, nh, 32])
        sb = sn[own_s][:, 0:32][:, None, :].to_broadcast([128, nh, 32])
        o4 = qr[pr][:, out_cols:out_cols + n].rearrange("p (h i two) -> p h i two", two=2, i=32)
        h0 = o0 // 64
        A, B = ta[pr][:, h0:h0 + nh, :], tb_[pr][:, h0:h0 + nh, :]
        TT(P, A, t0, cb, ALU.mult, [bqn[pr], bcs[own_s]], [bta[pr]])
        TT(P, B, t1, sb, ALU.mult, [bqn[pr], bcs[own_s]], [btb[pr]], eng="pool")
        TT(P, o4[:, :, :, 0], A, B, ALU.subtract, [bta[pr], btb[pr]], [bqr[pr]])
        TT(P, A, t0, sb, ALU.mult, [bqn[pr], bcs[own_s]], [bta[pr]])
        TT(P, B, t1, cb, ALU.mult, [bqn[pr], bcs[own_s]], [btb[pr]], eng="pool")
        TT(P, o4[:, :, :, 1], A, B, ALU.add, [bta[pr], btb[pr]], [bqr[pr]])

    NTW = T // TW
    NTW_OWN = SH // TW
    csi = 0
    pending_tr = []
    for tt in range(NTW):
        s = tt % 2
        own = tt < NTW_OWN
        DMA(P, hn[s][:], HNTv[:, :, tt * TW:tt * TW + TW + 2], [b_hnt], [bhn1[s]])
        for su in range(NSUB):
            tok0 = tt * TW + su * 128
            c_s = csi % 2
            csi += 1
            DMA(P, cs[c_s][:], cosd[tok0:tok0 + 128, :], (), [bcs[c_s]])
            DMA(P, sn[c_s][:], sind[tok0:tok0 + 128, :], (), [bcs[c_s]])
            lcols = slice(1 + su * 128, 1 + (su + 1) * 128)
            if own:
                bq = rot(ROT1)
                for c in range(NCH):
                    MM(P, BK[bq][:, 0:512], hn[s][:, c, lcols], wb[:, c, C_AQ:C_AQ + 512], c == 0, c == NCH - 1, [bhn1[s], bwb], bk[bq])
            bkv = rot(ROT1)
            for c in range(NCH):
                MM(P, BK[bkv][:, 0:256], hn[s][:, c, lcols], wb[:, c, C_AK:C_AK + 256], c == 0, c == NCH - 1, [bhn1[s], bwb], bk[bkv])
            CP(P, v1[s][:, su, :].rearrange("p (g e) -> p g e", e=65)[:, :, 0:64],
               BK[bkv][:, 128:256].rearrange("p (g e) -> p g e", e=64), (), [bv1[s]], [bk[bkv]], eng="act")
            pr = c_s
            if own:
                qk_norm_rope(bq, 0, 8, gq, 0, c_s, pr, 0)
            qk_norm_rope(bkv, 0, 2, gk, 512, c_s, pr, 512)

            def do_tr(s=s, su=su, pr=pr, own=own):
                bt = rot(ROT1)
                ptv = BK[bt][:].bitcast(BF16)
                if own:
                    for h in range(8):
                        TR(P, ptv[0:64, h * 128:(h + 1) * 128], qr[pr][:, h * 64:(h + 1) * 64], identb[:], [bqr[pr], b_const], bk[bt])
                    CP(P, qT[s][:, :, su * 128:(su + 1) * 128], ptv[0:64, :].rearrange("p (h t) -> p h t", t=128), (), [bqT[s]], [bk[bt]], eng="act")
                    bt = rot(ROT1)
                    ptv = BK[bt][:].bitcast(BF16)
                for g in range(2):
                    TR(P, ptv[0:64, g * 128:(g + 1) * 128], qr[pr][:, 512 + g * 64:512 + (g + 1) * 64], identb[:], [bqr[pr], b_const], bk[bt])
                CP(P, kTt[s][:, :, su * 128:(su + 1) * 128], ptv[0:64, 0:256].rearrange("p (h t) -> p h t", t=128), (), [bkT[s]], [bk[bt]], eng="act")
            if pending_tr:
                pending_tr.pop()()
            pending_tr.append(do_tr)
            bm = rot(ROT1)
            for c in range(NCH):
                MM(P, BK[bm][:, 0:512], hn[s][:, c, lcols], wb[:, c, C_MV:C_MV + 512], c == 0, c == NCH - 1, [bhn1[s], bwb], bk[bm])
            CP(P, mv[s][:, su, :].rearrange("p (h e) -> p h e", e=129)[:, :, 0:128],
               BK[bm][:, 0:512].rearrange("p (h e) -> p h e", e=128), (), [bmv[s]], [bk[bm]], eng="act")
            if own:
                bo = rot(ROT1)
                for c in range(NCH):
                    MM(P, BK[bo][:, 0:512], hn[s][:, c, lcols], wb[:, c, C_MO:C_MO + 512], c == 0, c == NCH - 1, [bhn1[s], bwb], bk[bo])
                ACT(P, so[s][:, su, :], BK[bo][:, 0:512], AF.Sigmoid, (), [bso[s]], [bk[bo]])
        if pending_tr:
            pending_tr.pop()()
        if own:
            DMA(P, QR[:, :, tt * TW:(tt + 1) * TW], qT[s][:], [bqT[s]], [b_p1out], q="pool")
            DMA(P, SO[:, tt * NSUB:(tt + 1) * NSUB, :], so[s][:], [bso[s]], [b_p1out], q="pool")
        DMA(P, KR[:, :, tt * TW:(tt + 1) * TW], kTt[s][:], [bkT[s]], [b_p1out], q="pool")
        DMA(P, V1[:, tt * NSUB:(tt + 1) * NSUB, :], v1[s][:], [bv1[s]], [b_p1out], q="pool")
        DMA(P, MV[:, tt * NSUB:(tt + 1) * NSUB, :], mv[s][:], [bmv[s]], [b_p1out], q="pool")
        for j in range(8):
            isq = j < 4
            if isq and not own:
                continue
            base = C_MQ if isq else C_MK
            jj = j % 4
            bc = rot(ROT1)
            n = 0
            for tap in range(3):
                for c in range(NCH):
                    MM(P, BK[bc][:, 0:TW], wb[:, c, base + tap * 512 + jj * 128:base + tap * 512 + (jj + 1) * 128],
                       hn[s][:, c, tap:tap + TW], n == 0, n == 23, [bhn1[s], bwb], bk[bc])
                    n += 1
            if isq:
                ACT(P, mqT[s][:, jj, :], BK[bc][:, 0:TW], AF.Silu, [bcb], [bmq[s]], [bk[bc]], bias=cbT[:, j:j + 1])
            else:
                ACT(P, mkf[:], BK[bc][:, 0:TW], AF.Silu, [bcb], [bmkf], [bk[bc]], bias=cbT[:, j:j + 1])
                TS(P, mkT[s][:, jj, :], mkf[:], 128.0 ** -0.5, None, ALU.mult, reads=[bmkf], writes=[bmk[s]])
        for su in range(NSUB):
            bt = rot(ROT1)
            ptv = BK[bt][:].bitcast(BF16)
            for h in range(4):
                TR(P, ptv[:, h * 128:(h + 1) * 128], mkT[s][:, h, su * 128:(su + 1) * 128], identb[:], [bmk[s], b_const], bk[bt])
            CP(P, mkt[s][:, su, :], ptv[:, 0:512], (), [bmkt[s]], [bk[bt]], eng="act")
        if own:
            DMA(P, MQT[:, :, tt * TW:(tt + 1) * TW], mqT[s][:], [bmq[s]], [b_p1out], q="pool")
        DMA(P, MKT[:, :, tt * TW:(tt + 1) * TW], mkT[s][:], [bmk[s]], [b_p1out], q="pool")
        DMA(P, MK[:, tt * NSUB:(tt + 1) * NSUB, :], mkt[s][:], [bmkt[s]], [b_p1out], q="pool")
        bgp = rot(ROT1)
        for c in range(NCH):
            MM(P, BK[bgp][0:8, 0:TW], wb[:, c, C_GI:C_GI + 8], hn[s][:, c, 1:1 + TW], c == 0, c == NCH - 1, [bhn1[s], bwb], bk[bgp])
        ACT(P, gi[s][:], BK[bgp][0:8, 0:TW], AF.Identity, [bgb], [bgi[s]], [bk[bgp]], bias=ib[:])
        bgp = rot(ROT1)
        for c in range(NCH):
            MM(P, BK[bgp][0:8, 0:TW], wb[:, c, C_GF:C_GF + 8], hn[s][:, c, 1:1 + TW], c == 0, c == NCH - 1, [bhn1[s], bwb], bk[bgp])
        ACT(P, gf[s][:], BK[bgp][0:8, 0:TW], AF.Exp, [bgb], [bgf[s]], [bk[bgp]], bias=fbn[:], scale=-1.0)
        ACT(P, gf[s][:], gf[s][:], AF.Ln, [bgf[s]], [bgf[s]], bias=1.0)
        TS(P, gf[s][:], gf[s][:], -1.0, None, ALU.mult, reads=[bgf[s]], writes=[bgf[s]])
        DMA(P, GIA[:, tt * TW:(tt + 1) * TW], gi[s][0:4, :], [bgi[s]], [b_p1out], q="pool")
        DMA(P, GFA[:, tt * TW:(tt + 1) * TW], gf[s][0:4, :], [bgf[s]], [b_p1out], q="pool")
        DMA(P, GIB[:, tt * TW:(tt + 1) * TW], gi[s][4:8, :], [bgi[s]], [b_p1out], q="pool")
        DMA(P, GFB[:, tt * TW:(tt + 1) * TW], gf[s][4:8, :], [bgf[s]], [b_p1out], q="pool")
    P.end()

    P.begin()
    RT2 = (NTT % 2 == 0)
    KRs = P.sbuf([128, 2, NTT // 2, 128], BF16) if RT2 else P.sbuf([64, 2, T], BF16)
    V1s = P.sbuf([128, NTT, 130], BF16)
    VpA = P.sbuf([128, NTT, 2, 128], BF16)
    VpB = P.sbuf([128, NTT, 2, 128], BF16)
    bld = Buf()
    bvp = Buf()
    for g in range(2):
        if RT2:
            kv = KR[:, g, :].rearrange("d (m two c) -> d m two c", two=2, c=128)
            DMA(P, KRs[0:64, g, :, :], kv[:, :, 0, :], (), [bld])
            DMA(P, KRs[64:128, g, :, :], kv[:, :, 1, :], (), [bld])
        else:
            DMA(P, KRs[:, g, :], KR[:, g, :], (), [bld])
    DMA(P, V1s[:], V1, (), [bld])
    bvpB = Buf()
    MSET(P, VpA[:], 0.0, [bvp])
    MSET(P, VpB[:], 0.0, [bvpB])
    for g in range(2):
        CP(P, VpA[:, :, g, 0:65], V1s[:, :, g * 65:(g + 1) * 65], [bld, bvp], [bvp])
        CP(P, VpB[:, :, g, 64:128], V1s[:, :, g * 65:g * 65 + 64], [bld, bvpB], [bvpB], eng="act")
        CP(P, VpB[:, :, g, 0:1], V1s[:, :, g * 65 + 64:g * 65 + 65], [bld, bvpB], [bvpB], eng="act")
    onesA = P.sbuf([128, 64], F32)
    onesB = P.sbuf([1, 128], F32)
    bones = Buf()
    MSET(P, onesA[:], 1.0, [bones])
    MSET(P, onesB[:], 0.0, [bones])
    MSET(P, onesB[:, 64:128], 1.0, [bones])
    QB = 512 if SH >= 512 else SH
    NQB = SH // QB
    QRs = [P.sbuf([128, 8, QB], BF16) for _ in range(2)]
    bqr = [Buf() for _ in range(2)]
    KB = 2 if NTT % 2 == 0 else 1
    pt = [P.sbuf([128, KB, QB], BF16) for _ in range(3)]
    bpt = [Buf() for _ in range(3)]
    rcp = [P.sbuf([128, QB], F32) for _ in range(2)]
    brcp = [Buf() for _ in range(2)]
    rbs = [P.sbuf([128, QB], F32) for _ in range(2)]
    brbs = [Buf() for _ in range(2)]
    mixs = [P.sbuf([128, 4, QB], BF16) for _ in range(2)]
    bmix = [Buf() for _ in range(2)]
    b_mixt = Buf()
    OBK = 6
    RBK = 7
    dst = [P.sbuf([128, 1024], F32) for _ in range(2)]
    bdst = [Buf() for _ in range(2)]
    dbf = [P.sbuf([128, 1024], BF16) for _ in range(2)]
    bdbf = [Buf() for _ in range(2)]
    dT = [P.sbuf([128, 1024], BF16) for _ in range(2)]
    bdT = [Buf() for _ in range(2)]
    ust = [P.sbuf([128, 1024], F32) for _ in range(2)]
    bust = [Buf() for _ in range(2)]
    ubf = [P.sbuf([128, 1024], BF16) for _ in range(2)]
    bubf = [Buf() for _ in range(2)]
    b_tab = Buf()

    def prep_block(i):
        s = i % 2
        DMA(P, dst[s][:], down[i * 128:(i + 1) * 128, :], (), [bdst[s]])
        DMA(P, ust[s][:], up[i * 128:(i + 1) * 128, :], (), [bust[s]])
        CP(P, dbf[s][:], dst[s][:], [bdst[s]], [bdbf[s]], eng="dve")
        pv = BK[RBK][:].bitcast(BF16)
        for c in range(8):
            TR(P, pv[:, c * 128:(c + 1) * 128], dbf[s][:, c * 128:(c + 1) * 128], identb[:], [bdbf[s], b_const], bk[RBK])
        CP(P, dT[s][:], pv, (), [bdT[s]], [bk[RBK]])
        DMA(P, DT[:, i, :], dT[s][:], [bdT[s]], [b_tab], q="pool")
        CP(P, ubf[s][:], ust[s][:], [bust[s]], [bubf[s]], eng="dve")
        DMA(P, UB[:, i, :], ubf[s][:], [bubf[s]], [b_tab], q="pool")

    NKB = NTT // KB
    iters = [(qb, h, kb) for qb in range(NQB) for h in range(8) for kb in range(NKB)]
    NIT = len(iters)

    def load_q(qb):
        for h in range(8):
            DMA(P, QRs[qb % 2][0:64, h, :], QR[:, h, qb * QB:(qb + 1) * QB], (), [bqr[qb % 2]])
            if RT2:
                DMA(P, QRs[qb % 2][64:128, h, :], QR[:, h, qb * QB:(qb + 1) * QB], (), [bqr[qb % 2]])

    def emit_S(n):
        qb, h, kb = iters[n]
        g = h // 4
        p_ = n % 3
        for j in range(KB):
            kc = kb * KB + j
            sb_ = 2 * p_ + j
            if RT2:
                rows = slice(64 * (kc % 2), 64 * (kc % 2) + 64)
                MM(P, BK[sb_][:, 0:QB], KRs[rows, g, kc // 2, :], QRs[qb % 2][rows, h, :], True, True, [bld, bqr[qb % 2]], bk[sb_])
            else:
                MM(P, BK[sb_][:, 0:QB], KRs[:, g, kc * 128:(kc + 1) * 128], QRs[qb % 2][0:64, h, :], True, True, [bld, bqr[qb % 2]], bk[sb_])
        if KB == 2 and QB == 512:
            src = P.big[:, 2 * p_ * 512:(2 * p_ + 2) * 512]
            ACT(P, pt[p_][:].rearrange("p k q -> p (k q)"), src, AF.Exp, (), [bpt[p_]], [bk[2 * p_], bk[2 * p_ + 1]])
        else:
            for j in range(KB):
                ACT(P, pt[p_][:, j, :], BK[2 * p_ + j][:, 0:QB], AF.Exp, (), [bpt[p_]], [bk[2 * p_ + j]])

    def emit_PV(n):
        qb, h, kb = iters[n]
        g = h // 4
        hi = qb * 8 + h
        ob = OBK
        Vp = VpA if h % 2 == 0 else VpB
        p_ = n % 3
        for j in range(KB):
            kc = kb * KB + j
            MM(P, BK[ob][:, 0:QB], Vp[:, kc, g, :], pt[p_][:, j, :], kc == 0, kc == NTT - 1, [bpt[p_], bvp if h % 2 == 0 else bvpB], bk[ob])
        if kb == NKB - 1:
            r_ = hi % 2
            a_s = qb % 2
            rb = RBK
            if h % 2 == 0:
                P.op("dve", lambda e, o=rcp[r_][64:65, :], i=BK[ob][64:65, 0:QB]: e.reciprocal(out=o, in_=i), (), [brcp[r_]], [bk[ob]])
                MM(P, BK[rb][0:64, 0:QB], onesA[64:65, 0:64], rcp[r_][64:65, :], True, True, [brcp[r_], bones], bk[rb])
                rows = slice(0, 64)
            else:
                P.op("dve", lambda e, o=rcp[r_][0:1, :], i=BK[ob][0:1, 0:QB]: e.reciprocal(out=o, in_=i), (), [brcp[r_]], [bk[ob]])
                MM(P, BK[rb][:, 0:QB], onesB[0:1, :], rcp[r_][0:1, :], True, True, [brcp[r_], bones], bk[rb])
                rows = slice(64, 128)
            CP(P, rbs[r_][rows, :], BK[rb][rows, 0:QB], (), [brbs[r_]], [bk[rb]])
            TT(P, mixs[a_s][rows, h // 2, :], BK[ob][rows, 0:QB], rbs[r_][rows, :], ALU.mult, [brbs[r_]], [bmix[a_s]], [bk[ob]])
            if h == 7:
                DMA(P, MIXT[:, 0:4, qb * QB:(qb + 1) * QB], mixs[a_s][:], [bmix[a_s]], [b_mixt], q="pool")

    load_q(0)
    if NQB > 1:
        load_q(1)
    for n in range(min(2, NIT)):
        emit_S(n)
    nprep = 0
    for n in range(NIT):
        qb, h, kb = iters[n]
        if n + 2 < NIT:
            emit_S(n + 2)
        emit_PV(n)
        if h == 7 and kb == NKB - 1 and qb + 2 < NQB:
            load_q(qb + 2)
        while nprep < 128 and nprep * NIT <= n * 128:
            prep_block(nprep)
            nprep += 1
    while nprep < 128:
        prep_block(nprep)
        nprep += 1
    P.end()

    P.begin()
    maskA = P.sbuf([128, 128], F32)
    maskB = P.sbuf([128, 128], F32)
    bmask = Buf()
    dif = P.sbuf([128, 128], F32)
    P.op("pool", lambda e: e.iota(dif[:], pattern=[[1, 128]], base=0, channel_multiplier=-1,
                                   allow_small_or_imprecise_dtypes=True), (), [bmask])
    P.op("dve", lambda e: e.tensor_single_scalar(out=maskA[:], in_=dif[:], scalar=0.0, op=ALU.is_ge), [bmask], [bmask])
    P.op("dve", lambda e: e.tensor_single_scalar(out=maskB[:], in_=dif[:], scalar=0.0, op=ALU.is_le), [bmask], [bmask])
    ogb = P.sbuf([128, 512], F32)
    bog = Buf()
    DMA(P, ogb[:], og.partition_broadcast(128), (), [bog])
    ones4 = P.sbuf([4, 128], F32)
    MSET(P, ones4[:], 1.0, [bog])

    ones1 = P.sbuf([1, 128], F32)
    MSET(P, ones1[:], 1.0, [bog])

    def gate_prep(GI_d, GF_d, NC, reverse):
        rows = 4 * NC
        RT = min(128, rows)
        ntile = rows // RT
        hpt = RT // NC
        betaT = P.sbuf([128, rows], F32)
        clmT = P.sbuf([128, rows], F32)
        decB = P.sbuf([128, rows], F32)
        bT = Buf()
        for k in range(ntile):
            h0 = k * hpt
            gi_ = P.sbuf([RT, 128], F32)
            gf_ = P.sbuf([RT, 128], F32)
            bb = Buf()
            for hh in range(hpt):
                DMA(P, gi_[hh * NC:(hh + 1) * NC, :], GI_d[h0 + hh, 0:NC * 128].rearrange("(c t) -> c t", t=128), (), [bb])
                DMA(P, gf_[hh * NC:(hh + 1) * NC, :], GF_d[h0 + hh, 0:NC * 128].rearrange("(c t) -> c t", t=128), (), [bb])
            one_ = P.sbuf([RT, 128], F32)
            MSET(P, one_[:], 1.0, [bb])
            bc_ = P.sbuf([RT, 128], F32)
            if reverse:
                P.op("dve", lambda e, o=bc_[:, ::-1], a=one_[:], b=gf_[:, ::-1]: e.tensor_tensor_scan(out=o, data0=a, data1=b, initial=0.0,
                                                                                                  op0=ALU.mult, op1=ALU.add), [bb], [bb])
            else:
                P.op("dve", lambda e, o=bc_[:], a=one_[:], b=gf_[:]: e.tensor_tensor_scan(out=o, data0=a, data1=b, initial=0.0,
                                                                                          op0=ALU.mult, op1=ALU.add), [bb], [bb])
            a_ = P.sbuf([RT, 128], F32)
            TT(P, a_[:], gi_[:], bc_[:], ALU.subtract, [bb], [bb])
            amax = P.sbuf([RT, 1], F32)
            RED(P, amax[:], a_[:], ALU.max, [bb], [bb])
            btc = bc_[:, 0:1] if reverse else bc_[:, 127:128]
            pb_ = rot([5, 6])
            TR(P, BK[pb_][0:1, 0:RT], amax[:, 0:1], identf[0:RT, 0:RT], [bb, b_const], bk[pb_])
            TR(P, BK[pb_][0:1, 128:128 + RT], btc, identf[0:RT, 0:RT], [bb, b_const], bk[pb_])
            row = P.sbuf([1, 2, 128], F32)
            CP(P, row[:, :, 0:RT], BK[pb_][0:1, 0:256].rearrange("p (a r) -> p a r", a=2)[:, :, 0:RT], (), [bb], [bk[pb_]])
            mnext = P.sbuf([1, RT], F32)
            for hh in range(hpt):
                sl = slice(hh * NC, (hh + 1) * NC)
                if reverse:
                    P.op("dve", lambda e, o=mnext[:, sl][:, ::-1], a=row[:, 0, sl][:, ::-1], b=row[:, 1, sl][:, ::-1]:
                         e.tensor_tensor_scan(out=o, data0=a, data1=b, initial=0.0, op0=ALU.max, op1=ALU.add), [bb], [bb])
                else:
                    P.op("dve", lambda e, o=mnext[:, sl], a=row[:, 0, sl], b=row[:, 1, sl]:
                         e.tensor_tensor_scan(out=o, data0=a, data1=b, initial=0.0, op0=ALU.max, op1=ALU.add), [bb], [bb])
            Mc = P.sbuf([1, RT], F32)
            TT(P, Mc[:], mnext[:], row[:, 1, 0:RT], ALU.subtract, [bb], [bb])
            mst = P.sbuf([1, RT], F32)
            MSET(P, mst[:], 0.0, [bb])
            if NC > 1:
                for hh in range(hpt):
                    if reverse:
                        CP(P, mst[:, hh * NC:(hh + 1) * NC - 1], mnext[:, hh * NC + 1:(hh + 1) * NC], [bb], [bb])
                    else:
                        CP(P, mst[:, hh * NC + 1:(hh + 1) * NC], mnext[:, hh * NC:(hh + 1) * NC - 1], [bb], [bb])
            dec = P.sbuf([1, RT], F32)
            TT(P, dec[:], mst[:], Mc[:], ALU.subtract, [bb], [bb])
            ACT(P, dec[:], dec[:], AF.Exp, [bb], [bb])
            TS(P, Mc[:], Mc[:], -1.0, None, ALU.mult, reads=[bb], writes=[bb])
            pb_ = rot([5, 6])
            TR(P, BK[pb_][0:RT, 0:1], Mc[0:1, 0:RT], identf[0:1, 0:1], [bb, b_const], bk[pb_])
            nMc = P.sbuf([RT, 1], F32)
            CP(P, nMc[:], BK[pb_][0:RT, 0:1], (), [bb], [bk[pb_]])
            beta = P.sbuf([RT, 128], F32)
            ACT(P, beta[:], a_[:], AF.Exp, [bb], [bb], bias=nMc[:])
            clm = P.sbuf([RT, 128], F32)
            ACT(P, clm[:], bc_[:], AF.Exp, [bb], [bb], bias=nMc[:], scale=-1.0)
            for src, dstT in ((beta, betaT), (clm, clmT)):
                pb_ = rot([5, 6])
                TR(P, BK[pb_][:, 0:RT], src[:], identf[0:RT, 0:RT], [bb, b_const], bk[pb_])
                CP(P, dstT[:, k * RT:(k + 1) * RT], BK[pb_][:, 0:RT], (), [bT], [bk[pb_]])
            pb_ = rot([5, 6])
            MM(P, BK[pb_][:, 0:RT], ones1[:], dec[:], True, True, [bb, bog], bk[pb_])
            CP(P, decB[:, k * RT:(k + 1) * RT], BK[pb_][:, 0:RT], (), [bT], [bk[pb_]])
        return betaT, clmT, decB, bT, NC

    betaA, clmA, decA, bTA, NCA = gate_prep(GIA, GFA, NOWN, False)
    betaB, clmB, decB_, bTB, NCB = gate_prep(GIB, GFB, NTT, True)

    U = {}
    for d_ in "AB":
        for h in range(4):
            u = P.sbuf([128, 129], F32)
            bu = Buf()
            MSET(P, u[:], 0.0, [bu])
            U[(d_, h)] = (u, bu)
    hA = P.sbuf([128, NOWN, 512], F32)
    bhA = [Buf() for _ in range(NOWN)]
    NB = 3
    ld = {}
    for d_ in "AB":
        ld[d_] = dict(
            q=[P.sbuf([128, 4, 128], BF16) for _ in range(NB)], k=[P.sbuf([128, 4, 128], BF16) for _ in range(NB)],
            kt=[P.sbuf([128, 512], BF16) for _ in range(NB)], v=[P.sbuf([128, 4 * 129], BF16) for _ in range(NB)],
            b=[Buf() for _ in range(NB)])
    vb = [P.sbuf([128, 129], BF16) for _ in range(4)]
    bvb = [Buf() for _ in range(4)]
    stm = [P.sbuf([128, 128], BF16) for _ in range(4)]
    bstm = [Buf() for _ in range(4)]
    chat = [P.sbuf([128, 129], BF16) for _ in range(4)]
    bchat = [Buf() for _ in range(4)]
    den = [P.sbuf([128, 1], F32) for _ in range(4)]
    bden = [Buf() for _ in range(4)]
    den2 = [P.sbuf([128, 1], F32) for _ in range(4)]
    bden2 = [Buf() for _ in range(4)]
    hB = [P.sbuf([128, 512], F32) for _ in range(2)]
    bhB = [Buf() for _ in range(2)]
    sos = [P.sbuf([128, 512], BF16) for _ in range(2)]
    bsos = [Buf() for _ in range(2)]
    gso = P.sbuf([128, 512], F32)
    bgso = Buf()
    hs = P.sbuf([128, 512], F32)
    bhs = Buf()
    junk3 = P.sbuf([128, 128], F32)
    bj3 = Buf()
    ss3 = P.sbuf([128, 4], F32)
    bss3 = Buf()
    memt = P.sbuf([128, 512], BF16)
    bmemt = Buf()
    FB4 = 4 if NOWN % 4 == 0 else 1
    memT = [P.sbuf([128, 4, 128 * FB4], BF16) for _ in range(2)]
    bmemT = [Buf() for _ in range(2)]
    PROT = [0, 1, 2, 3, 4]
    cnt = {"A": 0, "B": 0, "x": 0, "f": 0}

    def step(d_, c, output):
        betaT, clmT, decT, NCd = (betaA, clmA, decA, NCA) if d_ == "A" else (betaB, clmB, decB_, NCB)
        bT = bTA if d_ == "A" else bTB
        mask = maskA if d_ == "A" else maskB
        L = ld[d_]
        s = cnt[d_] % NB
        cnt[d_] += 1
        bl = L["b"][s]
        if output:
            DMA(P, L["q"][s][:], MQT[:, :, c * 128:(c + 1) * 128], (), [bl])
            DMA(P, L["k"][s][:], MKT[:, :, c * 128:(c + 1) * 128], (), [bl])
        DMA(P, L["kt"][s][:], MK[:, c, :], (), [bl])
        DMA(P, L["v"][s][:], MV[:, c, :], (), [bl])
        for h in range(4):
            u, bu = U[(d_, h)]
            w = cnt["x"] % 4
            cnt["x"] += 1
            TS(P, vb[w][:], L["v"][s][:, h * 129:(h + 1) * 129], betaT[:, h * NCd + c:h * NCd + c + 1], None, ALU.mult, reads=[bl, bT], writes=[bvb[w]])
            if output:
                p1 = rot(PROT)
                MM(P, BK[p1][:, 0:128], L["k"][s][:, h, :], L["q"][s][:, h, :], True, True, [bl], bk[p1])
                TT(P, stm[w][:], BK[p1][:, 0:128], mask[:], ALU.mult, [bmask], [bstm[w]], [bk[p1]])
                TS(P, chat[w][:], u[:], decT[:, h * NCd + c:h * NCd + c + 1], None, ALU.mult, reads=[bu, bT], writes=[bchat[w]])
                p2 = rot(PROT)
                MM(P, BK[p2][:, 0:129], stm[w][:], vb[w][:], True, False, [bstm[w], bvb[w]], bk[p2])
                MM(P, BK[p2][:, 0:129], L["q"][s][:, h, :], chat[w][:], False, True, [bl, bchat[w]], bk[p2])
                TS(P, den[w][:], BK[p2][:, 128:129], clmT[:, h * NCd + c:h * NCd + c + 1], None, ALU.max, reads=[bT], writes=[bden[w]], psum=[bk[p2]])
                TS(P, den2[w][:], BK[p2][:, 128:129], -1.0, clmT[:, h * NCd + c:h * NCd + c + 1], ALU.mult, ALU.max, reads=[bT], writes=[bden2[w]], psum=[bk[p2]])
                TT(P, den[w][:], den[w][:], den2[w][:], ALU.max, [bden[w], bden2[w]], [bden[w]])
                RECIP(P, den[w][:], den[w][:], [bden[w]], [bden[w]])
                if d_ == "A":
                    TS(P, hA[:, c, h * 128:(h + 1) * 128], BK[p2][:, 0:128], den[w][:], None, ALU.mult, reads=[bden[w]], writes=[bhA[c]], psum=[bk[p2]])
                else:
                    fs = cnt["f"] % 2
                    TS(P, hB[fs][:, h * 128:(h + 1) * 128], BK[p2][:, 0:128], den[w][:], None, ALU.mult, reads=[bden[w]], writes=[bhB[fs]], psum=[bk[p2]])
            p3 = rot(PROT)
            MM(P, BK[p3][:, 0:129], L["kt"][s][:, h * 128:(h + 1) * 128], vb[w][:], True, True, [bl, bvb[w]], bk[p3])
            TS(P, u[:], u[:], decT[:, h * NCd + c:h * NCd + c + 1], None, ALU.mult, reads=[bu, bT], writes=[bu])
            TT(P, u[:], u[:], BK[p3][:, 0:129], ALU.add, [bu], [bu], [bk[p3]])

    def finalize(c):
        fs = cnt["f"] % 2
        cnt["f"] += 1
        DMA(P, sos[fs][:], SO[:, c, :], (), [bsos[fs]])
        TT(P, hs[:], hA[:, c, :], hB[fs][:], ALU.add, [bhA[c], bhB[fs]], [bhs])
        for h in range(4):
            ACT(P, junk3[:], hs[:, h * 128:(h + 1) * 128], AF.Square, [bhs], [bj3, bss3], accum_out=ss3[:, h:h + 1])
        ACT(P, ss3[:], ss3[:], AF.Sqrt, [bss3, b_const], [bss3], bias=epst[:], scale=1.0 / 128)
        RECIP(P, ss3[:], ss3[:], [bss3], [bss3])
        TT(P, gso[:], ogb[:], sos[fs][:], ALU.mult, [bog, bsos[fs]], [bgso], eng="pool")
        TT(P, hs[:].rearrange("p (h e) -> p h e", e=128), hs[:].rearrange("p (h e) -> p h e", e=128),
           ss3[:, :, None].to_broadcast([128, 4, 128]), ALU.mult, [bhs, bss3], [bhs])
        TT(P, memt[:], hs[:], gso[:], ALU.mult, [bhs, bgso], [bmemt])
        tb = rot([5, 6])
        ptv = BK[tb][:].bitcast(BF16)
        for h in range(4):
            TR(P, ptv[:, h * 128:(h + 1) * 128], memt[:, h * 128:(h + 1) * 128], identb[:], [bmemt, b_const], bk[tb])
        fg = (c // FB4) % 2
        fsub = c % FB4
        CP(P, memT[fg][:, :, fsub * 128:(fsub + 1) * 128], ptv[:, 0:512].rearrange("p (h t) -> p h t", t=128), (), [bmemT[fg]], [bk[tb]], eng="act")
        if fsub == 0:
            DMA(P, MIXT[:, 4:8, c * 128:(c + FB4) * 128], memT[fg][:], [bmemT[fg]], [b_mixt], q="pool")

    for k in range(NOWN):
        step("A", k, True)
        step("B", NTT - 1 - k, False)
    for k in range(NOWN):
        c = NOWN - 1 - k
        step("B", c, True)
        finalize(c)
    P.end()

    P.begin()
    wo = P.sbuf([128, NCH, D], BF16)
    bwo = Buf()
    wst4 = [P.sbuf([128, D], F32) for _ in range(2)]
    bws4 = [Buf() for _ in range(2)]
    for c in range(NCH):
        s = c % 2
        DMA(P, wst4[s][:], w_out[c * 128:(c + 1) * 128, :], (), [bws4[s]])
        CP(P, wo[:, c, :], wst4[s][:], [bws4[s]], [bwo], eng="act" if c % 2 else "dve")
    g2T = P.sbuf([128, NCH], F32)
    bg2 = Buf()
    load_colvec(n2g, NCH, g2T[:], bg2, 7)
    MB4 = 4 if NOWN % 4 == 0 else 1
    mx_ = [P.sbuf([128, NCH, 128 * MB4], BF16) for _ in range(2)]
    bmx = [Buf() for _ in range(2)]
    x4 = [P.sbuf([128, D], F32) for _ in range(2)]
    bx4 = [Buf() for _ in range(2)]
    h4 = [P.sbuf([128, D], F32) for _ in range(2)]
    bh4 = [Buf() for _ in range(2)]
    junk4 = P.sbuf([128, D], BF16)
    bj4 = Buf()
    ss4 = [P.sbuf([128, 1], F32) for _ in range(2)]
    bss4 = [Buf() for _ in range(2)]
    hs4 = [P.sbuf([128, D], BF16) for _ in range(2)]
    bhs4 = [Buf() for _ in range(2)]
    xn2 = [P.sbuf([128, NCH, 128 * MB4], BF16) for _ in range(2)]
    bxn2 = [Buf() for _ in range(2)]
    b_y = Buf()
    b_xn2t = Buf()
    for i in range(NOWN):
        s = i % 2
        mg = (i // MB4) % 2
        msub = i % MB4
        if msub == 0:
            DMA(P, mx_[mg][:], MIXT[:, :, i * 128:(i + MB4) * 128], [b_mixt], [bmx[mg]])
        DMA(P, x4[s][:], x[i * 128:(i + 1) * 128, :], (), [bx4[s]])
        for hh in range(2):
            pb_ = rot([0, 1, 2, 3])
            for c in range(NCH):
                MM(P, BK[pb_][:, 0:512], mx_[mg][:, c, msub * 128:(msub + 1) * 128], wo[:, c, hh * 512:(hh + 1) * 512], c == 0, c == NCH - 1,
                   [bmx[mg], bwo], bk[pb_])
            TT(P, h4[s][:, hh * 512:(hh + 1) * 512], BK[pb_][:, 0:512], x4[s][:, hh * 512:(hh + 1) * 512], ALU.add, [bx4[s]], [bh4[s]], [bk[pb_]])
        DMA(P, y[i * 128:(i + 1) * 128, :], h4[s][:], [bh4[s]], [b_y], q="pool")
        ACT(P, junk4[:], h4[s][:], AF.Square, [bh4[s]], [bj4, bss4[s]], accum_out=ss4[s][:])
        ACT(P, ss4[s][:], ss4[s][:], AF.Sqrt, [bss4[s], b_const], [bss4[s]], bias=epst[:], scale=1.0 / D)
        RECIP(P, ss4[s][:], ss4[s][:], [bss4[s]], [bss4[s]])
        ACT(P, hs4[s][:], h4[s][:], AF.Copy, [bh4[s], bss4[s]], [bhs4[s]], scale=ss4[s][:])
        tb = rot([4, 5, 6])
        pv = BK[tb][:].bitcast(BF16).rearrange("p (c t) -> p c t", c=NCH)
        for c in range(NCH):
            TR(P, pv[:, c, :], hs4[s][:, c * 128:(c + 1) * 128], identb[:], [bhs4[s], b_const], bk[tb])
        TT(P, xn2[mg][:, :, msub * 128:(msub + 1) * 128], pv, g2T[:, :, None].to_broadcast([128, NCH, 128]), ALU.mult, [bg2], [bxn2[mg]], [bk[tb]])
        if msub == MB4 - 1:
            DMA(P, XN2T[:, :, (i - MB4 + 1) * 128:(i + 1) * 128], xn2[mg][:], [bxn2[mg]], [b_xn2t], q="pool")
    P.end()

    build_peer(P, nc, dict(SH=SH, down=down, up=up, DT=DT, UB=UB, wq=wq, subk=subk, XN2T=XN2T, y=y, identb=identb, identf=identf,
                           iotab=iotab, iotaf=iotaf, b_const=b_const, rot=rot, kscr=kscr))
    P.finish()
    return nc


def build_peer(P, nc, L):
    SH = L["SH"]
    down, up, DT, UB, wq, subk, XN2T, y = L["down"], L["up"], L["DT"], L["UB"], L["wq"], L["subk"], L["XN2T"], L["y"]
    identb, identf, iotaf, b_const, rot = L["identb"], L["identf"], L["iotaf"], L["b_const"], L["rot"]
    BK, bk = P.banks, P.bk
    NOWN = SH // 128
    kscr = L["kscr"]
    ISEL = nc.dram_tensor("ISEL", [128, SH], F32, kind=kscr).ap()
    JSEL = nc.dram_tensor("JSEL", [128, SH], F32, kind=kscr).ap()
    GATE = nc.dram_tensor("GATE", [128, SH], F32, kind=kscr).ap()

    P.begin()
    wqb = P.sbuf([128, 8, 2048], BF16)
    bwq = Buf()
    wqs = [P.sbuf([128, 2048], F32) for _ in range(2)]
    bwqs = [Buf() for _ in range(2)]
    for c in range(8):
        s = c % 2
        DMA(P, wqs[s][:], wq[c * 128:(c + 1) * 128, :], (), [bwqs[s]])
        CP(P, wqb[:, c, :], wqs[s][:], [bwqs[s]], [bwq], eng="act" if c % 2 else "dve")
    sks = P.sbuf([128, 16, 128], F32)
    bsk = Buf()
    DMA(P, sks[:], subk.rearrange("a n d -> n a d"), (), [bsk])
    skb = P.sbuf([128, 16, 128], BF16)
    CP(P, skb[:], sks[:], [bsk], [bsk])
    subkT = P.sbuf([128, 16, 128], BF16)
    bskT = Buf()
    for half in range(2):
        b = rot([4, 5, 6, 7])
        pv = BK[b][:].bitcast(BF16)
        for a in range(8):
            TR(P, pv[:, a * 128:(a + 1) * 128], skb[:, half * 8 + a, :], identb[:], [bsk, b_const], bk[b])
        CP(P, subkT[:, half * 8:(half + 1) * 8, :].rearrange("p a n -> p (a n)"), pv, (), [bskT], [bk[b]])
    xs_ = [P.sbuf([128, 8, 128], BF16) for _ in range(2)]
    bxs_ = [Buf() for _ in range(2)]
    sel = [P.sbuf([128, 3, 128], F32) for _ in range(2)]
    bsel = [Buf() for _ in range(2)]
    selT = [P.sbuf([128, 3, 128], F32) for _ in range(2)]
    bselT = [Buf() for _ in range(2)]
    b_route = Buf()
    iota16 = iotaf[:, 0:16]

    class Slot:
        pass

    slots = []
    for _ in range(2):
        S_ = Slot()
        S_.qh = P.sbuf([128, 16, 128], BF16)
        S_.sc = P.sbuf([128, 16, 128], F32)
        S_.sc2 = P.sbuf([128, 16, 128], F32)
        S_.v = P.sbuf([128, 16, 16], F32)
        S_.ix = P.sbuf([128, 16, 16], U32)
        S_.ixf = P.sbuf([128, 16, 16], F32)
        S_.cand = P.sbuf([128, 8, 256], F32)
        S_.cand2 = P.sbuf([128, 8, 256], F32)
        S_.tv = P.sbuf([128, 8, 16], F32)
        S_.pos = P.sbuf([128, 8, 16], U32)
        S_.k12 = P.sbuf([128, 2, 128], U32)
        S_.k12f = P.sbuf([128, 2, 128], F32)
        S_.oh = [P.sbuf([128, 8, 16, 16], F32) for _ in range(2)]
        S_.ez = P.sbuf([128, 8], F32)
        S_.bqh, S_.btop, S_.btop2, S_.bcand, S_.bk12, S_.bez = Buf(), Buf(), Buf(), Buf(), Buf(), Buf()
        S_.boh = [Buf(), Buf()]
        S_.rowb1 = [[Buf() for _ in range(5)] for _ in range(16)]
        S_.rowb2 = [[Buf() for _ in range(5)] for _ in range(8)]
        slots.append(S_)

    def top16(src3, nrow, vals, idxs, scratch3, bsrc, brow):
        for r in range(nrow):
            P.op("dve", lambda e, o=vals[:, r, 0:8], i=src3[:, r, :]: e.max(out=o, in_=i), [bsrc], [brow[r][0]])
        yield
        for r in range(nrow):
            P.op("dve", lambda e, o=idxs[:, r, 0:8], m=vals[:, r, 0:8], i=src3[:, r, :]: e.max_index(out=o, in_max=m, in_values=i),
                 [bsrc, brow[r][0]], [brow[r][1]])
        yield
        for r in range(nrow):
            P.op("dve", lambda e, o=scratch3[:, r, :], m=vals[:, r, 0:8], i=src3[:, r, :]: e.match_replace(out=o, in_to_replace=m, in_values=i, imm_value=-1e30),
                 [bsrc, brow[r][0]], [brow[r][2]])
        yield
        for r in range(nrow):
            P.op("dve", lambda e, o=vals[:, r, 8:16], i=scratch3[:, r, :]: e.max(out=o, in_=i), [brow[r][2]], [brow[r][3]])
        yield
        for r in range(nrow):
            P.op("dve", lambda e, o=idxs[:, r, 8:16], m=vals[:, r, 8:16], i=scratch3[:, r, :]: e.max_index(out=o, in_max=m, in_values=i),
                 [brow[r][2], brow[r][3]], [brow[r][4]])
        yield

    def route(su):
        s = su % 2
        Q = slots[s]
        DMA(P, xs_[s][:], XN2T[:, :, su * 128:(su + 1) * 128], (), [bxs_[s]])
        for hp in range(16):
            b = rot([4, 5, 6, 7])
            for c in range(8):
                MM(P, BK[b][:, 0:128], wqb[:, c, hp * 128:(hp + 1) * 128], xs_[s][:, c, :], c == 0, c == 7, [bwq, bxs_[s]], bk[b])
            CP(P, Q.qh[:, hp, :], BK[b][:, 0:128], (), [Q.bqh], [bk[b]], eng="act")
        yield
        for q4 in range(4):
            b = rot([0, 1, 2, 3])
            for a in range(4):
                hp = q4 * 4 + a
                MM(P, BK[b][:, a * 128:(a + 1) * 128], Q.qh[:, hp, :], subkT[:, hp, :], True, True, [Q.bqh, bskT], bk[b], skip=True)
            CP(P, Q.sc[:, q4 * 4:(q4 + 1) * 4, :].rearrange("p a n -> p (a n)"), BK[b][:, 0:512], (), [Q.btop], [bk[b]], eng="act")
        yield
        yield from top16(Q.sc, 16, Q.v, Q.ix, Q.sc2, Q.btop, Q.rowb1)
        all1 = [b_ for r in range(16) for b_ in Q.rowb1[r]]
        CP(P, Q.ixf[:], Q.ix[:], all1, [Q.btop2])
        v4 = Q.v[:].rearrange("p (h two) k -> p h two k", two=2)
        TT(P, Q.cand[:].rearrange("p h (a b) -> p h a b", b=16), v4[:, :, 0, :, None].to_broadcast([128, 8, 16, 16]),
           v4[:, :, 1, None, :].to_broadcast([128, 8, 16, 16]), ALU.add, all1, [Q.bcand])
        yield
        yield from top16(Q.cand, 8, Q.tv, Q.pos, Q.cand2, Q.bcand, Q.rowb2)
        all2 = [b_ for r in range(8) for b_ in Q.rowb2[r]]
        P.op("dve", lambda e: e.tensor_single_scalar(out=Q.k12[:, 0, :], in_=Q.pos[:].rearrange("p h k -> p (h k)"), scalar=4, op=ALU.logical_shift_right), all2, [Q.bk12])
        P.op("dve", lambda e: e.tensor_single_scalar(out=Q.k12[:, 1, :], in_=Q.pos[:].rearrange("p h k -> p (h k)"), scalar=15, op=ALU.bitwise_and), all2, [Q.bk12])
        yield
        CP(P, Q.k12f[:], Q.k12[:], [Q.bk12], [Q.bk12])
        g3 = sel[s][:, 2, :].rearrange("p (h k) -> p h k", k=16)
        TT(P, g3, Q.tv[:], Q.tv[:, :, 0:1].to_broadcast([128, 8, 16]), ALU.subtract, all2, [bsel[s]])
        ACT(P, sel[s][:, 2, :], sel[s][:, 2, :], AF.Exp, [bsel[s]], [bsel[s]])
        yield
        ix4 = Q.ixf[:].rearrange("p (h two) k -> p h two k", two=2)
        for w_ in range(2):
            kf = Q.k12f[:, w_, :].rearrange("p (h k) -> p h k", k=16)
            TT(P, Q.oh[w_][:], kf[:, :, :, None].to_broadcast([128, 8, 16, 16]), iota16[:, None, None, :].to_broadcast([128, 8, 16, 16]), ALU.is_equal,
               [Q.bk12, b_const], [Q.boh[w_]])
            TT(P, Q.oh[w_][:], Q.oh[w_][:], ix4[:, :, w_, None, :].to_broadcast([128, 8, 16, 16]), ALU.mult, [Q.boh[w_], Q.btop2], [Q.boh[w_]], eng="pool")
            yield
        RED(P, Q.ez[:], g3, ALU.add, [bsel[s]], [Q.bez])
        RECIP(P, Q.ez[:], Q.ez[:], [Q.bez], [Q.bez])
        TT(P, g3, g3, Q.ez[:, :, None].to_broadcast([128, 8, 16]), ALU.mult, [bsel[s], Q.bez], [bsel[s]])
        yield
        for w_ in range(2):
            RED(P, sel[s][:, w_, :], Q.oh[w_][:].rearrange("p h k kk -> p (h k) kk"), ALU.add, [Q.boh[w_]], [bsel[s]])
        yield
        b = rot([4, 5, 6, 7])
        for w_ in range(3):
            TR(P, BK[b][:, w_ * 128:(w_ + 1) * 128], sel[s][:, w_, :], identf[:], [bsel[s], b_const], bk[b])
        CP(P, selT[s][:].rearrange("p a t -> p (a t)"), BK[b][:, 0:384], (), [bselT[s]], [bk[b]], eng="act")
        DMA(P, ISEL[:, su * 128:(su + 1) * 128], selT[s][:, 0, :], [bselT[s]], [b_route], q="pool")
        DMA(P, JSEL[:, su * 128:(su + 1) * 128], selT[s][:, 1, :], [bselT[s]], [b_route], q="pool")
        DMA(P, GATE[:, su * 128:(su + 1) * 128], selT[s][:, 2, :], [bselT[s]], [b_route], q="pool")

    active = []
    nxt = 0
    while nxt < NOWN or active:
        while len(active) < 2 and nxt < NOWN:
            if active and nxt % 2 == active[0][0] % 2:
                break
            active.append((nxt, route(nxt)))
            nxt += 1
        for item in list(active):
            try:
                next(item[1])
            except StopIteration:
                active.remove(item)
    P.end()

    P.begin()
    TG = 256 if SH >= 256 else SH
    NTS = TG // 128
    NG = SH // TG
    TB = 16
    NBT = TG // TB
    IB = 2
    NTB = 4
    xg = [P.sbuf([128, 8, TG], BF16) for _ in range(2)]
    bxg = [Buf() for _ in range(2)]
    st_ = [P.sbuf([128, 3, TG], F32) for _ in range(2)]
    bst = [Buf() for _ in range(2)]
    Aoh = [P.sbuf([128, TB, 128], BF16) for _ in range(2)]
    bA = [Buf() for _ in range(2)]
    Boh = [P.sbuf([128, TB, 128], BF16) for _ in range(2)]
    bB = [Buf() for _ in range(2)]
    Gall = [P.sbuf([128, TG, 128], BF16) for _ in range(2)]
    bG = [Buf() for _ in range(2)]
    dtb = [P.sbuf([128, IB, 1024], BF16) for _ in range(NTB)]
    ubb = [P.sbuf([128, IB, 1024], BF16) for _ in range(NTB)]
    bdt = [Buf() for _ in range(NTB)]
    bub = [Buf() for _ in range(NTB)]
    LA = 4
    NGB = LA + 2
    ge = [P.sbuf([128, TG], BF16) for _ in range(NGB)]
    bge = [Buf() for _ in range(NGB)]
    Wt = [P.sbuf([128, TG], BF16) for _ in range(NGB)]
    bW = [Buf() for _ in range(NGB)]
    hrow = [P.sbuf([128, 512], F32) for _ in range(4)]
    bhr = [Buf() for _ in range(4)]
    b_yout = Buf()
    cn = {"A": 0, "G": 0, "h": 0}
    abank = {}
    iotab = L["iotab"]

    def load_group(tg):
        gs = tg % 2
        t_lo = tg * TG
        DMA(P, xg[gs][:], XN2T[:, :, t_lo:t_lo + TG], (), [bxg[gs]])
        DMA(P, st_[gs][:, 0, :], ISEL[:, t_lo:t_lo + TG], (), [bst[gs]])
        DMA(P, st_[gs][:, 1, :], JSEL[:, t_lo:t_lo + TG], (), [bst[gs]])
        DMA(P, st_[gs][:, 2, :], GATE[:, t_lo:t_lo + TG], (), [bst[gs]])

    free_banks = [4, 5, 6, 7]

    def next_bank():
        assert free_banks, "PSUM ring exhausted"
        return free_banks.pop(0)

    def g_part(tg, bi, part, gate_eng="dve"):
        gs = tg % 2
        a_s = bi % 2
        hf = TB // 2
        half = part % 2
        t0_ = bi * TB + half * hf
        sl = slice(half * hf, (half + 1) * hf)
        io3 = iotaf[:, None, :].to_broadcast([128, hf, 128])
        if part < 2:
            TT(P, Aoh[a_s][:, sl, :], io3, st_[gs][:, 0, t0_:t0_ + hf, None].to_broadcast([128, hf, 128]), ALU.is_equal, [bst[gs], b_const], [bA[a_s]])
            TT(P, Aoh[a_s][:, sl, :], Aoh[a_s][:, sl, :], st_[gs][:, 2, t0_:t0_ + hf, None].to_broadcast([128, hf, 128]), ALU.mult, [bst[gs], bA[a_s]], [bA[a_s]],
               eng=gate_eng)
        else:
            TT(P, Boh[a_s][:, sl, :], io3, st_[gs][:, 1, t0_:t0_ + hf, None].to_broadcast([128, hf, 128]), ALU.is_equal, [bst[gs], b_const], [bB[a_s]])

    def g_round(tg, bi, rd):
        gs = tg % 2
        a_s = bi % 2
        t4 = rd * 4
        b = next_bank()
        cn["G"] += 1
        for t in range(4):
            MM(P, BK[b][:, t * 128:(t + 1) * 128], Boh[a_s][:, t4 + t, :], Aoh[a_s][:, t4 + t, :], True, True, [bA[a_s], bB[a_s]], bk[b], skip=True)
        tt0 = bi * TB + t4
        CP(P, Gall[gs][:, tt0:tt0 + 4, :].rearrange("p t i -> p (t i)"), BK[b][:, 0:512], (), [bG[gs]], [bk[b]], eng="act")
        free_banks.append(b)

    def g_build(tg, bi):
        for part in range(4):
            g_part(tg, bi, part, gate_eng="pool")

    def g_mm(tg, bi):
        for rd in range(TB // 4):
            g_round(tg, bi, rd)

    NBLK = 128 // IB

    def load_tab(tg, blk):
        ts_ = (tg * NBLK + blk) % NTB
        DMA(P, dtb[ts_][:], DT[:, blk * IB:(blk + 1) * IB, :], (), [bdt[ts_]])
        DMA(P, ubb[ts_][:], UB[:, blk * IB:(blk + 1) * IB, :], (), [bub[ts_]])

    def emit_aT(tg, i):
        gs = tg % 2
        ts_ = (tg * NBLK + i // IB) % NTB
        ab = next_bank()
        abank[(tg, i)] = ab
        for c in range(8):
            MM(P, BK[ab][:, 0:TG], dtb[ts_][:, i % IB, c * 128:(c + 1) * 128], xg[gs][:, c, :], c == 0, c == 7, [bdt[ts_], bxg[gs]], bk[ab])

    def emit_mid(tg, i):
        gs = tg % 2
        es = i % NGB
        ab = abank.pop((tg, i))
        ACT(P, ge[es][:], BK[ab][:, 0:TG], AF.Gelu, (), [bge[es]], [bk[ab]])
        free_banks.append(ab)
        TT(P, Wt[es][:], ge[es][:], Gall[gs][:, :, i], ALU.mult, [bge[es], bG[gs]], [bW[es]], eng="dve")

    def emit_up(tg, i):
        es = i % NGB
        ts_ = (tg * NBLK + i // IB) % NTB
        for ts2 in range(NTS):
            for dh in range(2):
                ob = ts2 * 2 + dh
                MM(P, BK[ob][:, 0:512], Wt[es][:, ts2 * 128:(ts2 + 1) * 128], ubb[ts_][:, i % IB, dh * 512:(dh + 1) * 512], i == 0, i == 127,
                   [bW[es], bub[ts_]], bk[ob])

    load_group(0)
    for bi in range(NBT):
        g_build(0, bi)
        g_mm(0, bi)
    for tg in range(NG):
        gs = tg % 2
        t_lo = tg * TG
        if tg + 1 < NG:
            load_group(tg + 1)
        if tg == 0:
            for blk in range(min(NTB - 1, NBLK)):
                load_tab(tg, blk)
        for k in range(LA):
            emit_aT(tg, k)
            emit_mid(tg, k)
        for i in range(128):
            if i % IB == 0 and i // IB + NTB - 1 < NBLK:
                load_tab(tg, i // IB + NTB - 1)
            emit_up(tg, i)
            if i + LA < 128:
                emit_aT(tg, i + LA)
                emit_mid(tg, i + LA)
            if tg + 1 < NG:
                IV = 128 // NBT
                bi_ = i // IV
                ph = i % IV
                if IV >= 8:
                    if ph % 2 == 0:
                        g_part(tg + 1, bi_, ph // 2)
                    elif bi_ >= 1:
                        g_round(tg + 1, bi_ - 1, ph // 2)
                elif ph == IV - 1:
                    if bi_ >= 1:
                        g_mm(tg + 1, bi_ - 1)
                    g_build(tg + 1, bi_)
                if i == 127:
                    g_mm(tg + 1, NBT - 1)
        if tg + 1 < NG:
            for blk in range(min(NTB - 1, NBLK)):
                load_tab(tg + 1, blk)
        for ts2 in range(NTS):
            r0 = t_lo + ts2 * 128
            for dh in range(2):
                hs_ = cn["h"] % 4
                cn["h"] += 1
                ob = ts2 * 2 + dh
                DMA(P, hrow[hs_][:], y[r0:r0 + 128, dh * 512:(dh + 1) * 512], (), [bhr[hs_]])
                TT(P, hrow[hs_][:], hrow[hs_][:], BK[ob][:, 0:512], ALU.add, [bhr[hs_]], [bhr[hs_]], [bk[ob]])
                DMA(P, y[r0:r0 + 128, dh * 512:(dh + 1) * 512], hrow[hs_][:], [bhr[hs_]], [b_yout], q="pool")
    P.end()


_NC_CACHE = {}


def rope_tables(pos):
    pos = np.asarray(pos)
    row = (pos // 64).astype(np.float32)
    col = (pos % 64).astype(np.float32)
    inv = (10000.0 ** (-np.arange(0, 32, 2, dtype=np.float32) / 32)).astype(np.float32)
    ang = np.concatenate([row[:, None] * inv, col[:, None] * inv], axis=-1).astype(np.float32)
    return np.cos(ang).astype(np.float32), np.sin(ang).astype(np.float32)


def make_in_maps(inp, debug=False):
    x = np.asarray(inp["x"], np.float32)
    B, S, _ = x.shape
    SH = S // 2
    f = lambda k: np.asarray(inp[k], np.float32)[0]
    w_in = f("w_in")
    perm = np.arange(2832)
    perm_flip = perm.copy()
    for base in (2816, 2824):
        perm_flip[base:base + 4] = np.arange(base + 4, base + 8)
        perm_flip[base + 4:base + 8] = np.arange(base, base + 4)
    convw = f("ml_conv_w")
    ibias = f("ml_igate_bias")
    fbias = f("ml_fgate_bias")
    common = {
        "n1g": f("norm1_gain").reshape(8, 128), "n2g": f("norm2_gain").reshape(8, 128),
        "qg": f("att_q_gain"), "kg": f("att_k_gain"), "convb": f("ml_conv_b").reshape(8, 128),
        "og": f("ml_out_gain").reshape(512), "w_out": f("w_out"), "wq": f("peer_w_query"),
        "subk": f("peer_sub_keys").reshape(16, 128, 128), "down": f("peer_down"), "up": f("peer_up"),
    }
    maps = []
    for c in range(2 * B):
        b, hf = c // 2, c % 2
        m = dict(common)
        if hf == 0:
            m["x"] = np.ascontiguousarray(x[b])
            pos = np.arange(S)
            m["w_in"] = w_in
            m["convw"] = convw
            m["ibias"] = np.ascontiguousarray(ibias.reshape(8, 1))
            m["fbias"] = np.ascontiguousarray(fbias.reshape(8, 1))
        else:
            m["x"] = np.ascontiguousarray(x[b, ::-1])
            pos = np.arange(S)[::-1]
            m["w_in"] = np.ascontiguousarray(w_in[:, perm_flip])
            m["convw"] = np.ascontiguousarray(convw[::-1])
            m["ibias"] = np.ascontiguousarray(ibias[::-1].reshape(8, 1))
            m["fbias"] = np.ascontiguousarray(fbias[::-1].reshape(8, 1))
        cs, sn = rope_tables(pos)
        m["cos"], m["sin"] = cs, sn
        maps.append(m)
    return maps, SH


def kernel(**inp):
    maps, SH = make_in_maps(inp)
    if SH not in _NC_CACHE:
        _NC_CACHE[SH] = build(SH)
    nc = _NC_CACHE[SH]
    res = run_bass_kernel_spmd(nc, maps, core_ids=list(range(len(maps))))
    B = len(maps) // 2
    out = np.zeros((B, 2 * SH, D), np.float32)
    for c in range(2 * B):
        b, hf = c // 2, c % 2
        yc = np.asarray(res.results[c]["y"], np.float32)
        if hf == 0:
            out[b, :SH] = yc
        else:
            out[b, SH:] = yc[::-1]
    return out
```
